# Optimizing a Trainium2 kernel written in Bass

```python
import math
import jax, jax.numpy as jnp
from jax import lax
import numpy as np

D_MODEL = 2048
BATCH = 4
SEQ = 8192
DEPTH = 1

HEAD_DIM = 128
A_HEADS = 8
DILATION_GROUPS = ((128, 1), (512, 4), (2048, 16))
N_GROUPS = 3
A_BLOCK = 64
NEG_INF = -1e30
B_QK_HEADS = 8
B_V_HEADS = 16
B_DK = 128
B_DV = 128
CONV_W = 5
CHUNK = 64
RMS_EPS = 1e-6
N_EXPERTS = 32
TOP_K = 4
D_FF = D_MODEL
SWIGLU_ALPHA = 1.702
SWIGLU_LIMIT = 7.0
MOE_BLOCK = 512
PLE_DIM = 256
DEEPNORM_ALPHA = (2 * DEPTH) ** 0.25
DEEPNORM_BETA = (8 * DEPTH) ** -0.25
LN_EPS = 1e-5
A_QKV = N_GROUPS * A_HEADS * HEAD_DIM
B_QK = B_QK_HEADS * B_DK
B_VZ = B_V_HEADS * B_DV
B_GATES = 4 * B_V_HEADS
SPLITS = (A_QKV, A_QKV, A_QKV, B_QK, B_QK, B_VZ, B_VZ, B_GATES, 2 * D_MODEL)
N_IN_COLS = sum(SPLITS)

kernel_name = 'hybrid_dilated_attn_deltanet_moe_encoder'


def layer_norm(x, g, b):
    x = x.astype(jnp.float32)
    mu = jnp.mean(x, axis=-1, keepdims=True)
    var = jnp.mean(jnp.square(x - mu), axis=-1, keepdims=True)
    return (x - mu) * lax.rsqrt(var + LN_EPS) * g.astype(jnp.float32) + b.astype(jnp.float32)


def alibi_slopes():
    n = N_GROUPS * A_HEADS
    s = 2.0 ** (-8.0 * np.arange(1, n + 1) / n)
    return s.astype(np.float32).reshape(N_GROUPS, A_HEADS)


def dilated_window_attention(q, k, v, dilation, n_side, slopes):
    bsz, seq, heads, dh = q.shape
    r = dilation
    L = seq // r
    nb = -(-L // A_BLOCK)
    Lp = nb * A_BLOCK
    span = A_BLOCK + 2 * n_side

    def split(t):
        return t.reshape(bsz, L, r, heads, dh).transpose(0, 2, 1, 3, 4).reshape(bsz * r, L, heads, dh)

    qs = jnp.pad(split(q), ((0, 0), (0, Lp - L), (0, 0), (0, 0)))
    kpad = ((0, 0), (n_side, Lp - L + n_side), (0, 0), (0, 0))
    ks = jnp.pad(split(k), kpad)
    vs = jnp.pad(split(v), kpad)
    qb = qs.reshape(bsz * r, nb, A_BLOCK, heads, dh)
    key_idx = np.arange(nb)[:, None] * A_BLOCK + np.arange(span)[None, :]
    kb = ks[:, key_idx]
    vb = vs[:, key_idx]
    key_pos = key_idx - n_side
    key_valid = (key_pos >= 0) & (key_pos < L)
    delta = np.arange(span)[None, :] - n_side - np.arange(A_BLOCK)[:, None]
    mask = (np.abs(delta) <= n_side)[None] & key_valid[:, None, :]
    bias = -(slopes[:, None, None] * (r * np.abs(delta)).astype(np.float32)[None])
    s = jnp.einsum('znqhd,znkhd->znhqk', qb.astype(jnp.float32), kb.astype(jnp.float32)) * (dh ** -0.5)
    s = jnp.where(mask[None, :, None], s + jnp.asarray(bias)[None, None], NEG_INF)
    lse = jax.nn.logsumexp(s, axis=-1)
    pr = jnp.exp(s - lse[..., None])
    o = jnp.einsum('znhqk,znkhd->znqhd', pr, vb.astype(jnp.float32))
    o = o.reshape(bsz * r, Lp, heads, dh)[:, :L]
    lse = lse.transpose(0, 1, 3, 2).reshape(bsz * r, Lp, heads)[:, :L]

    def merge(t):
        t = t.reshape((bsz, r, L) + t.shape[2:])
        return jnp.swapaxes(t, 1, 2).reshape((bsz, seq) + t.shape[3:])

    return merge(o), merge(lse)


def chunk_gated_delta_rule(q, k, v, beta, g):
    bsz, seq, h, dk = q.shape
    dv = v.shape[-1]
    n = seq // CHUNK

    def chunks(t):
        return t.reshape(bsz, n, CHUNK, h, -1).transpose(0, 3, 1, 2, 4)

    q, k, v = chunks(q), chunks(k), chunks(v)
    beta = beta.reshape(bsz, n, CHUNK, h).transpose(0, 3, 1, 2)
    g = jnp.cumsum(g.reshape(bsz, n, CHUNK, h).transpose(0, 3, 1, 2), axis=-1)
    incl = np.tril(np.ones((CHUNK, CHUNK), bool))
    strict = np.tril(np.ones((CHUNK, CHUNK), bool), -1)
    diff = g[..., :, None] - g[..., None, :]
    decay = jnp.where(incl, jnp.exp(jnp.where(incl, diff, 0.0)), 0.0)
    k_beta = k * beta[..., None]
    lower = jnp.where(strict, jnp.einsum('bhnck,bhnsk->bhncs', k_beta, k) * decay, 0.0)
    eye = jnp.eye(CHUNK, dtype=q.dtype)
    t_mat = lax.linalg.triangular_solve(lower + eye, jnp.broadcast_to(eye, lower.shape),
                                        left_side=True, lower=True)
    u = jnp.einsum('bhncs,bhnsv->bhncv', t_mat, v * beta[..., None])
    w = jnp.einsum('bhncs,bhnsk->bhnck', t_mat, k_beta * jnp.exp(g)[..., None])
    qk = jnp.where(incl, jnp.einsum('bhnck,bhnsk->bhncs', q, k) * decay, 0.0)
    q_decay = q * jnp.exp(g)[..., None]
    k_decay = k * jnp.exp(g[..., -1:] - g)[..., None]
    g_last = jnp.exp(g[..., -1])

    def step(state, xs):
        qd, kd, u_c, w_c, qk_c, gl = xs
        v_new = u_c - jnp.einsum('bhck,bhkv->bhcv', w_c, state)
        o = jnp.einsum('bhck,bhkv->bhcv', qd, state) + jnp.einsum('bhcs,bhsv->bhcv', qk_c, v_new)
        state = state * gl[..., None, None] + jnp.einsum('bhck,bhcv->bhkv', kd, v_new)
        return state, o

    xs = tuple(jnp.moveaxis(t, 2, 0) for t in (q_decay, k_decay, u, w, qk, g_last))
    state0 = jnp.zeros((bsz, h, dk, dv), q.dtype)
    _, o = lax.scan(step, state0, xs)
    return o.transpose(1, 0, 3, 2, 4).reshape(bsz, seq, h, dv)


def l2norm(t):
    return t * lax.rsqrt(jnp.sum(t * t, axis=-1, keepdims=True) + 1e-6)


def gated_deltanet(qkv, z, ba, conv_w, a_log, dt_bias, norm_w):
    bsz, seq, c = qkv.shape
    f32 = jnp.float32
    qkv = lax.conv_general_dilated(qkv, conv_w[:, None, :].astype(qkv.dtype), window_strides=(1,),
                                   padding=[(CONV_W // 2, CONV_W // 2)],
                                   dimension_numbers=('NWC', 'WIO', 'NWC'), feature_group_count=c)
    qkv = jax.nn.silu(qkv.astype(f32))
    q, k, v = jnp.split(qkv, [B_QK, 2 * B_QK], axis=-1)
    rep = B_V_HEADS // B_QK_HEADS
    q = jnp.repeat(l2norm(q.reshape(bsz, seq, B_QK_HEADS, B_DK)), rep, axis=2) * (B_DK ** -0.5)
    k = jnp.repeat(l2norm(k.reshape(bsz, seq, B_QK_HEADS, B_DK)), rep, axis=2)
    v = v.reshape(bsz, seq, B_V_HEADS, B_DV)
    b_f, a_f, b_b, a_b = jnp.split(ba.astype(f32), 4, axis=-1)
    a_log = a_log.astype(f32)
    dt_bias = dt_bias.astype(f32)
    g_f = -jnp.exp(a_log[0]) * jax.nn.softplus(a_f + dt_bias[0])
    g_b = -jnp.exp(a_log[1]) * jax.nn.softplus(a_b + dt_bias[1])
    o_f = chunk_gated_delta_rule(q, k, v, jax.nn.sigmoid(b_f), g_f)
    fl = lambda t: jnp.flip(t, axis=1)
    o_b = fl(chunk_gated_delta_rule(fl(q), fl(k), fl(v), fl(jax.nn.sigmoid(b_b)), fl(g_b)))
    o = o_f + o_b
    zg = jax.nn.silu(z.astype(f32).reshape(bsz, seq, B_V_HEADS, B_DV))
    o = o * lax.rsqrt(jnp.mean(o * o, axis=-1, keepdims=True) + RMS_EPS) * norm_w.astype(f32) * zg
    return o.reshape(bsz, seq, B_VZ)


def moe_swiglu(h, router_w, router_b, w_gate_up, b_gate_up, w_down, b_down):
    n_tok, d = h.shape
    f32 = jnp.float32
    logits = (h @ router_w).astype(f32) + router_b.astype(f32)
    top_val, top_idx = lax.top_k(logits, TOP_K)
    gates = jax.nn.softmax(top_val, axis=-1)
    n_assign = n_tok * TOP_K
    flat_e = top_idx.reshape(-1)
    order = jnp.argsort(flat_e)
    sorted_e = flat_e[order]
    sorted_tok = (order // TOP_K).astype(jnp.int32)
    sorted_gate = gates.reshape(-1)[order]
    counts = jnp.bincount(flat_e, length=N_EXPERTS)
    padded = (counts + MOE_BLOCK - 1) // MOE_BLOCK * MOE_BLOCK
    start = jnp.cumsum(counts) - counts
    pad_end = jnp.cumsum(padded)
    pad_start = pad_end - padded
    dest = pad_start[sorted_e] + jnp.arange(n_assign, dtype=jnp.int32) - start[sorted_e]
    n_pad = n_assign + N_EXPERTS * MOE_BLOCK
    n_blocks = n_pad // MOE_BLOCK
    buf_tok = jnp.zeros((n_pad,), jnp.int32).at[dest].set(sorted_tok)
    buf_gate = jnp.zeros((n_pad,), f32).at[dest].set(sorted_gate)
    block_start = jnp.arange(n_blocks, dtype=jnp.int32) * MOE_BLOCK
    block_expert = jnp.minimum(jnp.searchsorted(pad_end, block_start, side='right'), N_EXPERTS - 1)
    xs = h[buf_tok].reshape(n_blocks, MOE_BLOCK, d)

    def expert_block(args):
        xb, e = args
        gu = xb @ w_gate_up[e] + b_gate_up[e]
        gate = jnp.minimum(gu[:, 0::2], SWIGLU_LIMIT)
        up = jnp.clip(gu[:, 1::2], -SWIGLU_LIMIT, SWIGLU_LIMIT)
        act = (up + 1.0) * gate * jax.nn.sigmoid(gate * SWIGLU_ALPHA)
        return act @ w_down[e] + b_down[e]

    ys = lax.map(expert_block, (xs, block_expert)).reshape(n_pad, d)
    return jax.ops.segment_sum(ys.astype(f32) * buf_gate[:, None], buf_tok, num_segments=n_tok)


def setup_inputs(seed: int = 0) -> dict:
    key = jax.random.key(seed)
    ks = jax.random.split(key, 26)
    f32 = jnp.float32
    nrm = lambda k, shape, scale: jax.random.normal(k, shape, f32) * scale
    beta = DEEPNORM_BETA
    x = nrm(ks[0], (BATCH, SEQ, D_MODEL), 1.0)
    p = nrm(ks[1], (DEPTH, BATCH, SEQ, PLE_DIM), 1.0)
    col_scale = np.ones((N_IN_COLS,), np.float32)
    col_scale[2 * A_QKV:3 * A_QKV] = beta
    vb0 = 3 * A_QKV + 2 * B_QK
    col_scale[vb0:vb0 + B_VZ] = beta
    w_in = nrm(ks[2], (DEPTH, D_MODEL, N_IN_COLS), D_MODEL ** -0.5) * jnp.asarray(col_scale)
    b_gate = nrm(ks[3], (DEPTH, 2 * D_MODEL), 0.02)
    conv_w = nrm(ks[4], (DEPTH, CONV_W, 2 * B_QK + B_VZ), CONV_W ** -0.5)
    a_log = jnp.log(jax.random.uniform(ks[5], (DEPTH, 2, B_V_HEADS), f32, 1.0, 16.0))
    dt = jnp.exp(jax.random.uniform(ks[6], (DEPTH, 2, B_V_HEADS), f32, math.log(1e-3), math.log(1e-1)))
    dt_bias = dt + jnp.log(-jnp.expm1(-dt))
    dn_norm_w = 1.0 + nrm(ks[7], (DEPTH, B_DV), 0.02)
    w_branch_a = nrm(ks[8], (DEPTH, A_HEADS * HEAD_DIM, D_MODEL), (A_HEADS * HEAD_DIM) ** -0.5 * beta)
    w_branch_b = nrm(ks[9], (DEPTH, B_VZ, D_MODEL), B_VZ ** -0.5 * beta)
    w_out = nrm(ks[10], (DEPTH, D_MODEL, D_MODEL), D_MODEL ** -0.5 * beta)
    ln1_g = 1.0 + nrm(ks[11], (DEPTH, D_MODEL), 0.02)
    ln1_b = nrm(ks[12], (DEPTH, D_MODEL), 0.02)
    router_w = nrm(ks[13], (DEPTH, D_MODEL, N_EXPERTS), D_MODEL ** -0.5)
    router_b = nrm(ks[14], (DEPTH, N_EXPERTS), 0.01)
    w_gate_up = nrm(ks[15], (DEPTH, N_EXPERTS, D_MODEL, 2 * D_FF), D_MODEL ** -0.5)
    b_gate_up = nrm(ks[16], (DEPTH, N_EXPERTS, 2 * D_FF), 0.01)
    w_down = nrm(ks[17], (DEPTH, N_EXPERTS, D_FF, D_MODEL), D_FF ** -0.5 * beta)
    b_down = nrm(ks[18], (DEPTH, N_EXPERTS, D_MODEL), 0.01)
    w_ple = nrm(ks[19], (DEPTH, PLE_DIM, D_MODEL), PLE_DIM ** -0.5 * beta)
    w_ple_gate = nrm(ks[20], (DEPTH, D_MODEL, D_MODEL), D_MODEL ** -0.5)
    b_ple_gate = nrm(ks[21], (DEPTH, D_MODEL), 0.02)
    ln2_g = 1.0 + nrm(ks[22], (DEPTH, D_MODEL), 0.02)
    ln2_b = nrm(ks[23], (DEPTH, D_MODEL), 0.02)
    return {'x': x, 'p': p, 'w_in': w_in, 'b_gate': b_gate, 'conv_w': conv_w, 'a_log': a_log,
            'dt_bias': dt_bias, 'dn_norm_w': dn_norm_w, 'w_branch_a': w_branch_a,
            'w_branch_b': w_branch_b, 'w_out': w_out, 'ln1_g': ln1_g, 'ln1_b': ln1_b,
            'router_w': router_w, 'router_b': router_b, 'w_gate_up': w_gate_up,
            'b_gate_up': b_gate_up, 'w_down': w_down, 'b_down': b_down, 'w_ple': w_ple,
            'w_ple_gate': w_ple_gate, 'b_ple_gate': b_ple_gate, 'ln2_g': ln2_g, 'ln2_b': ln2_b}


def reference(x, p, w_in, b_gate, conv_w, a_log, dt_bias, dn_norm_w, w_branch_a, w_branch_b,
              w_out, ln1_g, ln1_b, router_w, router_b, w_gate_up, b_gate_up, w_down, b_down,
              w_ple, w_ple_gate, b_ple_gate, ln2_g, ln2_b):
    bsz, seq, _ = x.shape
    dt = x.dtype
    slopes = alibi_slopes()
    split_idx = np.cumsum(SPLITS)[:-1].tolist()
    for i in range(DEPTH):
        u = x @ w_in[i]
        qa, ka, va, qb, kb, vb, zb, bab, gpre = jnp.split(u, split_idx, axis=-1)
        hs = (bsz, seq, N_GROUPS, A_HEADS, HEAD_DIM)
        qa, ka, va = qa.reshape(hs), ka.reshape(hs), va.reshape(hs)
        outs, lses = [], []
        for gi, (win, dil) in enumerate(DILATION_GROUPS):
            o_g, l_g = dilated_window_attention(qa[:, :, gi], ka[:, :, gi], va[:, :, gi],
                                                dil, win // (2 * dil), slopes[gi])
            outs.append(o_g)
            lses.append(l_g)
        wts = jax.nn.softmax(jnp.stack(lses, axis=0), axis=0)
        o_a = jnp.einsum('gbsh,gbshd->bshd', wts, jnp.stack(outs, axis=0))
        o_a = o_a.reshape(bsz, seq, A_HEADS * HEAD_DIM).astype(dt)
        o_b = gated_deltanet(jnp.concatenate([qb, kb, vb], axis=-1), zb, bab, conv_w[i],
                             a_log[i], dt_bias[i], dn_norm_w[i]).astype(dt)
        gates = jax.nn.sigmoid(gpre.astype(jnp.float32) + b_gate[i].astype(jnp.float32))
        g_a, g_b = jnp.split(gates, 2, axis=-1)
        merged = g_a * (o_a @ w_branch_a[i]) + g_b * (o_b @ w_branch_b[i])
        mix = merged.astype(dt) @ w_out[i]
        x = layer_norm(DEEPNORM_ALPHA * x + mix, ln1_g[i], ln1_b[i]).astype(dt)
        y = moe_swiglu(x.reshape(bsz * seq, D_MODEL), router_w[i], router_b[i], w_gate_up[i],
                       b_gate_up[i], w_down[i], b_down[i]).reshape(bsz, seq, D_MODEL)
        ple = jax.nn.sigmoid((x @ w_ple_gate[i]).astype(jnp.float32) + b_ple_gate[i].astype(jnp.float32)) \
            * (p[i] @ w_ple[i]).astype(jnp.float32)
        x = layer_norm(DEEPNORM_ALPHA * x + y + ple, ln2_g[i], ln2_b[i]).astype(dt)
    return x
```

```python
from contextlib import ExitStack
import numpy as np
import concourse.bass as bass
import concourse.mybir as mybir
from concourse.bass_utils import run_bass_kernel_spmd

F32 = mybir.dt.float32
BF16 = mybir.dt.bfloat16
I32 = mybir.dt.int32
AF = mybir.ActivationFunctionType
ALU = mybir.AluOpType
AX = mybir.AxisListType

D_MODEL = 2048
KC = D_MODEL // 128
HEAD_DIM = 128
A_HEADS = 8
N_GROUPS = 3
DILS = (1, 4, 16)
NSIDE = 64
BIG = 30000.0


class Sched:
    EPOCH = 30000
    NDMA = 8

    def __init__(self, nc, gctx):
        self.nc = nc
        self.gctx = gctx
        self.ctx = None
        self.tag = "g"
        self.eng = {"pe": nc.tensor, "act": nc.scalar, "dve": nc.vector, "pool": nc.gpsimd, "sp": nc.sync}
        self.streams = {k: [] for k in self.eng}
        self.cnt = {k: 0 for k in self.eng}
        self.dcnt = {k: 0 for k in self.eng}
        self.sems = {}
        self.waited = {k: {} for k in self.eng}
        self.lastw = {}
        self.readers = {}
        self.dma_toks = {}
        self.nbuf = 0

    def begin(self, ctx, tag):
        self.ctx = ctx
        self.tag = tag
        return self

    def sb(self, name, shape, dtype):
        return self.ctx.enter_context(self.nc.sbuf_tensor(f"{self.tag}_{name}", list(shape), dtype))

    def ps(self, name, shape, dtype=F32):
        return self.ctx.enter_context(self.nc.psum_tensor(f"{self.tag}_{name}", list(shape), dtype))

    def getsem(self, key):
        s = self.sems.get(key)
        if s is None:
            nm = "sem_" + "_".join(str(x) for x in key)
            s = self.gctx.enter_context(self.nc.semaphore(nm))
            self.sems[key] = s
        return s

    @staticmethod
    def _norm(keys):
        out = []
        for k in keys:
            if isinstance(k, tuple) and len(k) == 4 and k[0] == "@":
                if Ring.CUR[(k[1], k[2])] != k[3]:
                    raise RuntimeError(f"stale ring buffer use {k}")
                k = k[:3]
            out.append(k)
        return out

    def op(self, eng, fn, reads=(), writes=(), dma=False):
        reads = self._norm(reads)
        writes = self._norm(writes)
        deps = {}

        def add(tok):
            if tok is None:
                return
            key, val = tok
            if deps.get(key, 0) < val:
                deps[key] = val

        for r in reads:
            add(self.lastw.get(r))
        for w in writes:
            add(self.lastw.get(w))
            for key, val in self.readers.get(w, {}).items():
                add((key, val))
        if dma:
            i = self.dcnt[eng]
            self.dcnt[eng] += 1
            slot = i % self.NDMA
            val = 16 * (i // self.NDMA + 1)
            key = ("d", eng, slot)
            if val > 16:
                add((key, val - 16))
            inc = 16
            self.dma_toks[key] = val
        else:
            n = self.cnt[eng]
            self.cnt[eng] += 1
            key = ("c", eng, n // self.EPOCH)
            val = n % self.EPOCH + 1
            inc = 1
        tok = (key, val)
        wd = self.waited[eng]
        waits = []
        for k2, v2 in deps.items():
            if wd.get(k2, 0) >= v2:
                continue
            wd[k2] = v2
            waits.append((self.getsem(k2), v2))
        self.streams[eng].append((waits, fn, self.getsem(key), inc))
        for r in reads:
            d = self.readers.setdefault(r, {})
            if d.get(key, 0) < val:
                d[key] = val
        for w in writes:
            self.lastw[w] = tok
            self.readers[w] = {}
        return tok

    def dma(self, eng, out, in_, reads=(), writes=(), **kw):
        return self.op(eng, lambda e: e.dma_start(out=out, in_=in_, **kw), reads, writes, dma=True)

    def emit(self):
        finals = []
        for k in self.eng:
            n = self.cnt[k]
            if n > 0:
                n -= 1
                finals.append((self.getsem(("c", k, n // self.EPOCH)), n % self.EPOCH + 1))
        for key, val in self.dma_toks.items():
            finals.append((self.getsem(key), val))
        streams = self.streams
        self.streams = {k: [] for k in self.eng}
        for k in self.eng:
            for s_, v_ in finals:
                pass
        for k in self.eng:
            for kk in self.eng:
                nn = self.cnt[kk]
                if nn > 0:
                    nn -= 1
                    key = ("c", kk, nn // self.EPOCH)
                    self.waited[k][key] = max(self.waited[k].get(key, 0), nn % self.EPOCH + 1)
            for key, val in self.dma_toks.items():
                self.waited[k][key] = max(self.waited[k].get(key, 0), val)

        def run(name, e):
            for waits, fn, sem, inc in streams[name]:
                for s, v in waits:
                    e.wait_ge(s, v)
                if fn is not None:
                    inst = fn(e)
                    inst.then_inc(sem, inc)
            for s, v in finals:
                e.wait_ge(s, v)

        with self.nc.Block() as block:
            @block.sync
            def _(e):
                run("sp", e)

            @block.tensor
            def _(e):
                run("pe", e)

            @block.scalar
            def _(e):
                run("act", e)

            @block.vector
            def _(e):
                run("dve", e)

            @block.gpsimd
            def _(e):
                run("pool", e)


class Ring:
    CUR = {}

    def __init__(self, items):
        self.items = items
        self.i = 0

    def next(self):
        idx = self.i % len(self.items)
        t, _ = self.items[idx]
        self.i += 1
        Ring.CUR[(id(self), idx)] = self.i
        return t, ("@", id(self), idx, self.i)


def phase_attention(sc, nc, S, xT3, w_att, abias, ident_d, o_aT, n_heads=4):
    scale = float(HEAD_DIM) ** -0.5
    with ExitStack() as ctx:
        sc.begin(ctx, "at")
        NT = S // 512
        NB = S // 128
        QT = sc.sb("QT", [128, S], BF16)
        KT = sc.sb("KT", [128, S], BF16)
        V = sc.sb("V", [128, NB, 128], BF16)
        acc = sc.sb("acc", [128, 2, S], F32)
        xR = Ring([(sc.sb(f"xb{i}", [128, KC, 512], BF16), f"xb{i}") for i in range(2)])
        wR = Ring([(sc.sb(f"wb{i}", [128, KC, 384], BF16), f"wb{i}") for i in range(2)])
        bR = Ring([(sc.sb(f"bb{i}", [128, 3, 128], F32), f"bb{i}") for i in range(2)])
        pR = Ring([(sc.sb(f"PT{i}", [128, 384], BF16), f"PT{i}") for i in range(3)])
        ident_f = sc.sb("identf", [128, 128], F32)
        ones_bf = sc.sb("ones", [128, 128], BF16)
        psP = Ring([(sc.ps(f"psP{i}", [128, 512]), f"psP{i}") for i in range(3)])
        psS = Ring([(sc.ps(f"psS{i}", [128, 512]), f"psS{i}") for i in range(2)])
        psO = Ring([(sc.ps(f"psO{i}", [128, 512]), f"psO{i}") for i in range(2)])

        sc.dma("sp", ident_f[:, :], ident_d, writes=["ident"])
        sc.op("pool", lambda e: e.memset(ones_bf[:, :], 1.0), writes=["ones"])

        for hl in range(n_heads):
            for g in range(N_GROUPS):
                r = DILS[g]
                L = S // r
                nbl = L // 128
                idx = hl * 3 + g
                w, wk = wR.next()
                sc.dma("pool", w[:, :, :], w_att[idx].rearrange("(kc p) n -> p kc n", p=128), writes=[wk])
                bt, bk = bR.next()
                sc.dma("sp", bt[:, :, :], abias[idx].rearrange("r k q -> k r q"), writes=[bk])
                for tt in range(NT):
                    xb, xk = xR.next()
                    sc.dma("pool", xb[:, :, :],
                           xT3[g][:, tt * 512:(tt + 1) * 512].rearrange("(kc p) t -> p kc t", p=128),
                           writes=[xk])
                    for which, dst in ((0, QT), (1, KT)):
                        ps, pk = psP.next()

                        def f(e, ps=ps, w=w, xb=xb, which=which):
                            for kc in range(KC):
                                inst = e.matmul(ps[:, :], lhsT=w[:, kc, which * 128:(which + 1) * 128],
                                                rhs=xb[:, kc, :], start=(kc == 0), stop=(kc == KC - 1))
                            return inst
                        sc.op("pe", f, reads=[wk, xk], writes=[pk])
                        dkey = ("QT" if which == 0 else "KT", tt)
                        if which == 0:
                            sc.op("act", lambda e, ps=ps, dst=dst, tt=tt: e.activation(
                                out=dst[:, tt * 512:(tt + 1) * 512], in_=ps[:, :], func=AF.Copy),
                                reads=[pk], writes=[dkey])
                        else:
                            sc.op("dve", lambda e, ps=ps, dst=dst, tt=tt: e.tensor_copy(
                                dst[:, tt * 512:(tt + 1) * 512], ps[:, :]),
                                reads=[pk], writes=[dkey])
                    ps, pk = psP.next()

                    def fv(e, ps=ps, w=w, xb=xb):
                        for sub in range(4):
                            for kc in range(KC):
                                inst = e.matmul(ps[:, sub * 128:(sub + 1) * 128],
                                                lhsT=xb[:, kc, sub * 128:(sub + 1) * 128],
                                                rhs=w[:, kc, 256:384], start=(kc == 0), stop=(kc == KC - 1))
                        return inst
                    sc.op("pe", fv, reads=[wk, xk], writes=[pk])
                    sc.op("dve", lambda e, ps=ps, tt=tt: e.tensor_copy(
                        V[:, tt * 4:(tt + 1) * 4, :], ps[:, :].rearrange("p (a d) -> p a d", a=4)),
                        reads=[pk], writes=[("V", tt)])
                for c in range(r):
                    for i0 in range(nbl):
                        blk = c * nbl + i0
                        rels = [rel for rel in (-1, 0, 1) if 0 <= i0 + rel < nbl]
                        lo = (rels[0] + 1) * 128
                        hi = (rels[-1] + 2) * 128
                        pss, psk = psS.next()
                        pt, ptk = pR.next()

                        def fs(e, pss=pss, blk=blk, rels=rels, bt=bt):
                            for rel in rels:
                                kt = blk + rel
                                o = pss[:, (rel + 1) * 128:(rel + 2) * 128]
                                e.matmul(o, lhsT=KT[:, kt * 128:(kt + 1) * 128],
                                         rhs=QT[:, blk * 128:(blk + 1) * 128], start=True, stop=False)
                                inst = e.matmul(o, lhsT=ident_f[:, :], rhs=bt[:, rel + 1, :],
                                                start=False, stop=True)
                            return inst
                        tks = sorted({(blk + rel) // 4 for rel in rels})
                        sc.op("pe", fs, reads=[("KT", t) for t in tks] + [("QT", blk // 4), "ident", bk],
                              writes=[psk])
                        sc.op("act", lambda e, pt=pt, pss=pss, lo=lo, hi=hi: e.activation(
                            out=pt[:, lo:hi], in_=pss[:, lo:hi], func=AF.Exp, scale=scale),
                            reads=[psk], writes=[ptk])
                        pso, pok = psO.next()

                        def fo(e, pso=pso, pt=pt, blk=blk, rels=rels):
                            for j, rel in enumerate(rels):
                                kt = blk + rel
                                e.matmul(pso[:, 0:128], lhsT=V[:, kt, :], rhs=pt[:, (rel + 1) * 128:(rel + 2) * 128],
                                         start=(j == 0), stop=(j == len(rels) - 1))
                            for j, rel in enumerate(rels):
                                inst = e.matmul(pso[:, 128:256], lhsT=ones_bf[:, :],
                                                rhs=pt[:, (rel + 1) * 128:(rel + 2) * 128],
                                                start=(j == 0), stop=(j == len(rels) - 1))
                            return inst
                        sc.op("pe", fo, reads=[("V", t) for t in tks] + [ptk, "ones"], writes=[pok])
                        st = i0 * 128 * r + c
                        asl = acc[:, :, st:st + 127 * r + 1:r]
                        psv = pso[:, 0:256].rearrange("p (a q) -> p a q", a=2)
                        if g == 0:
                            sc.op("dve", lambda e, asl=asl, psv=psv: e.tensor_copy(asl, psv),
                                  reads=[pok], writes=["acc"])
                        else:
                            sc.op("dve", lambda e, asl=asl, psv=psv: e.tensor_tensor(
                                out=asl, in0=asl, in1=psv, op=ALU.add), reads=[pok, "acc"], writes=["acc"])
            CH = min(S, 2048)
            for c0 in range(0, S, CH):
                sc.op("dve", lambda e, c0=c0: e.reciprocal(acc[:, 1, c0:c0 + CH], acc[:, 1, c0:c0 + CH]),
                      reads=["acc"], writes=["acc"])
                sc.op("dve", lambda e, c0=c0: e.tensor_tensor(
                    out=acc[:, 0, c0:c0 + CH], in0=acc[:, 0, c0:c0 + CH], in1=acc[:, 1, c0:c0 + CH],
                    op=ALU.mult), reads=["acc"], writes=["acc"])
            sc.dma("sp", o_aT[hl * 128:(hl + 1) * 128, :], acc[:, 0, :], reads=["acc"], writes=[("oaT", hl)])
        sc.emit()


def alibi_slopes():
    n = N_GROUPS * A_HEADS
    s = 2.0 ** (-8.0 * np.arange(1, n + 1) / n)
    return s.astype(np.float32).reshape(N_GROUPS, A_HEADS)


def make_abias(hf):
    scale = float(HEAD_DIM) ** -0.5
    slopes = alibi_slopes()
    out = np.zeros((12, 3, 128, 128), np.float32)
    k = np.arange(128)[:, None]
    q = np.arange(128)[None, :]
    for hl in range(4):
        for g in range(3):
            sl = float(slopes[g, hf * 4 + hl]) * DILS[g]
            for ri, rel in enumerate((-1, 0, 1)):
                delta = np.abs(rel * 128 + k - q).astype(np.float32)
                b = -(sl * delta) / scale
                out[hl * 3 + g, ri] = np.where(delta <= NSIDE, b, -1.0e6)
    return out


def permute_tokens(a, r):
    if r == 1:
        return a
    S = a.shape[-1]
    L = S // r
    return np.ascontiguousarray(a.reshape(a.shape[:-1] + (L, r)).swapaxes(-1, -2).reshape(a.shape))


DN_COLS = 3104
TW = 508


def phase_dn_proj(sc, nc, S, xT, w_dn, convT, dn_q, dn_k, dn_v, dn_zg, dn_gT):
    with ExitStack() as ctx:
        sc.begin(ctx, "dp")
        W = sc.sb("W", [128, KC, DN_COLS], BF16)
        cw = sc.sb("cw", [128, 16, 5], F32)
        ones_f = sc.sb("ones", [128, 128], F32)
        xR = Ring([(sc.sb(f"xb{i}", [128, KC, 512], BF16), f"xb{i}") for i in range(2)])
        cvR = Ring([(sc.sb(f"cv{i}", [128, TW], F32), f"cv{i}") for i in range(3)])
        sqR = Ring([(sc.sb(f"sq{i}", [128, TW], F32), f"sq{i}") for i in range(2)])
        rnR = Ring([(sc.sb(f"rn{i}", [128, TW], F32), f"rn{i}") for i in range(2)])
        obR = Ring([(sc.sb(f"ob{i}", [128, TW], BF16), f"ob{i}") for i in range(4)])
        ogR = Ring([(sc.sb(f"og{i}", [32, TW], F32), f"og{i}") for i in range(2)])
        psP = Ring([(sc.ps(f"psP{i}", [128, 512]), f"psP{i}") for i in range(4)])
        psN = Ring([(sc.ps(f"psN{i}", [128, 512]), f"psN{i}") for i in range(2)])

        for kc0 in range(0, KC, 4):
            sc.dma("pool", W[:, kc0:kc0 + 4, :],
                   w_dn[kc0 * 128:(kc0 + 4) * 128, :].rearrange("(kc p) n -> p kc n", p=128), writes=["W"])
        sc.dma("sp", cw[:, :, :], convT.rearrange("(c p) j -> p c j", p=128), writes=["cw"])
        sc.op("pool", lambda e: e.memset(ones_f[:, :], 1.0), writes=["ones"])
        epsc = sc.sb("epsc", [128, 1], F32)
        sc.op("pool", lambda e: e.memset(epsc[:, :], 1e-6), writes=["eps"])

        ntile = (S + TW - 1) // TW
        for i in range(ntile):
            t0 = TW * i
            n = min(TW, S - t0)
            lo = max(t0 - 2, 0)
            hi = min(t0 + 510, S)
            d0 = lo - (t0 - 2)
            xb, xk = xR.next()
            if d0 > 0 or (hi - lo) < 512:
                sc.op("pool", lambda e, xb=xb: e.memset(xb[:, :, :], 0.0), writes=[xk])
            sc.dma("pool", xb[:, :, d0:d0 + (hi - lo)],
                   xT[:, lo:hi].rearrange("(kc p) t -> p kc t", p=128), writes=[xk])
            for cc in range(25):
                ps, pk = psP.next()
                M = 128 if cc < 24 else 32

                def f(e, ps=ps, xb=xb, cc=cc, M=M):
                    for kc in range(KC):
                        inst = e.matmul(ps[0:M, :], lhsT=W[:, kc, cc * 128:cc * 128 + M], rhs=xb[:, kc, :],
                                        start=(kc == 0), stop=(kc == KC - 1))
                    return inst
                sc.op("pe", f, reads=["W", xk], writes=[pk])
                if cc < 16:
                    cv, ck = cvR.next()
                    sc.op("act", lambda e, cv=cv, ps=ps, cc=cc, n=n: e.activation(
                        out=cv[:, 0:n], in_=ps[:, 0:n], func=AF.Copy, scale=cw[:, cc, 0:1]),
                        reads=[pk, "cw"], writes=[ck])
                    for j in range(1, 5):
                        sc.op("dve", lambda e, cv=cv, ps=ps, cc=cc, n=n, j=j: e.scalar_tensor_tensor(
                            out=cv[:, 0:n], in0=ps[:, j:j + n], scalar=cw[:, cc, j:j + 1], in1=cv[:, 0:n],
                            op0=ALU.mult, op1=ALU.add), reads=[pk, "cw", ck], writes=[ck])
                    ob, ok = obR.next()
                    if cc < 8:
                        sc.op("act", lambda e, cv=cv, n=n: e.activation(out=cv[:, 0:n], in_=cv[:, 0:n], func=AF.Silu),
                              reads=[ck], writes=[ck])
                        sq, sk = sqR.next()
                        sc.op("act", lambda e, cv=cv, sq=sq, n=n: e.activation(out=sq[:, 0:n], in_=cv[:, 0:n], func=AF.Square),
                              reads=[ck], writes=[sk])
                        pn, pnk = psN.next()
                        sc.op("pe", lambda e, pn=pn, sq=sq, n=n: e.matmul(pn[:, 0:n], lhsT=ones_f[:, :], rhs=sq[:, 0:n],
                                                                           start=True, stop=True),
                              reads=[sk, "ones"], writes=[pnk])
                        rn, rk = rnR.next()
                        sc.op("act", lambda e, rn=rn, pn=pn, n=n: e.activation(
                            out=rn[:, 0:n], in_=pn[:, 0:n], func=AF.Ln, bias=epsc[:, 0:1]), reads=[pnk, "eps"], writes=[rk])
                        sc.op("act", lambda e, rn=rn, n=n: e.activation(
                            out=rn[:, 0:n], in_=rn[:, 0:n], func=AF.Exp, scale=-0.5), reads=[rk], writes=[rk])
                        scl = float(HEAD_DIM) ** -0.5 if cc < 4 else 1.0
                        sc.op("dve", lambda e, ob=ob, cv=cv, rn=rn, n=n, scl=scl: e.scalar_tensor_tensor(
                            out=ob[:, 0:n], in0=cv[:, 0:n], scalar=scl, in1=rn[:, 0:n], op0=ALU.mult, op1=ALU.mult),
                            reads=[ck, rk], writes=[ok])
                        dst = dn_q[cc] if cc < 4 else dn_k[cc - 4]
                    else:
                        sc.op("act", lambda e, cv=cv, ob=ob, n=n: e.activation(out=ob[:, 0:n], in_=cv[:, 0:n], func=AF.Silu),
                              reads=[ck], writes=[ok])
                        dst = dn_v[cc - 8]
                    sc.dma("sp", dst[:, t0:t0 + n], ob[:, 0:n], reads=[ok], writes=[("dst", cc)])
                elif cc < 24:
                    ob, ok = obR.next()
                    sc.op("act", lambda e, ps=ps, ob=ob, n=n: e.activation(out=ob[:, 0:n], in_=ps[:, 2:2 + n], func=AF.Silu),
                          reads=[pk], writes=[ok])
                    sc.dma("sp", dn_zg[cc - 16][:, t0:t0 + n], ob[:, 0:n], reads=[ok], writes=[("dst", cc)])
                else:
                    og, gk = ogR.next()
                    sc.op("dve", lambda e, ps=ps, og=og, n=n: e.tensor_copy(og[:, 0:n], ps[0:32, 2:2 + n]),
                          reads=[pk], writes=[gk])
                    sc.dma("sp", dn_gT[:, t0:t0 + n], og[:, 0:n], reads=[gk], writes=[("dst", cc)])
        sc.emit()


def phase_dn_main(sc, nc, S, dn_q, dn_k, dn_v, dn_gT, gparam, cmat, ident_d, dn_o, hk_list=(0, 1, 2, 3)):
    NT = S // 128
    with ExitStack() as octx:
        def osb(name, shape, dt):
            return octx.enter_context(nc.sbuf_tensor(f"dnp_{name}", list(shape), dt))
        A_beta = osb("beta", [128, NT, 16], F32)
        A_nbeta = osb("nbeta", [128, NT, 16], F32)
        A_gc = osb("gc", [128, NT, 16], F32)
        A_ngc = osb("ngc", [128, NT, 16], F32)
        A_bg = osb("bg", [128, NT, 16], F32)
        A_ekd = osb("ekd", [128, NT, 16], F32)
        A_gl = osb("gl", [128, NT, 16], F32)
        ident_f = osb("identf", [128, 128], F32)
        ident_b = osb("identb", [128, 128], BF16)
        ones_f = osb("onesf", [128, 128], F32)
        cm = osb("cm", [128, 6, 128], F32)

        with ExitStack() as ctx:
            sc.begin(ctx, "dg")
            gT = sc.sb("gT", [32, S], F32)
            sig = sc.sb("sig", [32, S], F32)
            gp = sc.sb("gp", [32, 2], F32)
            nA = sc.sb("nA", [32, 1], F32)
            tok = sc.sb("tok", [128, NT, 64], F32)
            gcall = sc.sb("gcall", [128, NT, 32], F32)
            tmp = sc.sb("tmp", [128, NT, 16], F32)
            psA = Ring([(sc.ps(f"psA{i}", [128, 512]), f"psA{i}") for i in range(2)])
            psB = Ring([(sc.ps(f"psB{i}", [128, 512]), f"psB{i}") for i in range(2)])
            sc.dma("sp", gT[:, :], dn_gT, writes=["gT"])
            sc.dma("sp", gp[:, :], gparam, writes=["gp"])
            sc.dma("sp", ident_f[:, :], ident_d, writes=["ident"])
            sc.dma("pool", ident_b[:, :], ident_d, writes=["identb"])
            sc.dma("sp", cm[:, :, :], cmat.rearrange("c k m -> k c m"), writes=["cm"])
            sc.op("pool", lambda e: e.memset(ones_f[:, :], 1.0), writes=["ones"])
            sc.op("act", lambda e: e.activation(out=nA[:, :], in_=gp[:, 1:2], func=AF.Exp), reads=["gp"], writes=["nA"])
            sc.op("dve", lambda e: e.tensor_scalar(out=nA[:, :], in0=nA[:, :], scalar1=-1.0, scalar2=None, op0=ALU.mult),
                  reads=["nA"], writes=["nA"])
            CH = min(S, 2048)
            for c0 in range(0, S, CH):
                sl = slice(c0, c0 + CH)
                sc.op("act", lambda e, sl=sl: e.activation(out=sig[:, sl], in_=gT[:, sl], func=AF.Sigmoid),
                      reads=["gT"], writes=["sig"])
                sc.op("act", lambda e, sl=sl: e.activation(out=gT[:, sl], in_=gT[:, sl], func=AF.Exp, bias=gp[:, 0:1]),
                      reads=["gT", "gp", "sig"], writes=["gT"])
                sc.op("act", lambda e, sl=sl: e.activation(out=gT[:, sl], in_=gT[:, sl], func=AF.Ln, bias=1.0),
                      reads=["gT"], writes=["gT"])
                sc.op("dve", lambda e, sl=sl: e.tensor_scalar(out=gT[:, sl], in0=gT[:, sl], scalar1=nA[:, 0:1], scalar2=None,
                                                               op0=ALU.mult), reads=["gT", "nA"], writes=["gT"])
            for n0 in range(0, NT, 8):
                nn = min(8, NT - n0)
                ps, pk = psA.next()

                def ft(e, ps=ps, n0=n0, nn=nn):
                    for a in range(nn):
                        n = n0 + a
                        e.transpose(ps[:, a * 64:a * 64 + 32], sig[0:32, n * 128:(n + 1) * 128], ident_f[0:32, 0:32])
                        inst = e.transpose(ps[:, a * 64 + 32:a * 64 + 64], gT[0:32, n * 128:(n + 1) * 128], ident_f[0:32, 0:32])
                    return inst
                sc.op("pe", ft, reads=["sig", "gT", "ident"], writes=[pk])
                sc.op("dve", lambda e, ps=ps, n0=n0, nn=nn: e.tensor_copy(
                    tok[:, n0:n0 + nn, :], ps[:, 0:nn * 64].rearrange("p (a c) -> p a c", c=64)),
                    reads=[pk], writes=[("tok", n0)])
            for n0 in range(0, NT, 16):
                nn = min(16, NT - n0)
                ps, pk = psB.next()

                def fc(e, ps=ps, n0=n0, nn=nn):
                    for a in range(nn):
                        n = n0 + a
                        e.matmul(ps[:, a * 32:a * 32 + 8], lhsT=cm[:, 0, :], rhs=tok[:, n, 40:48], start=True, stop=True)
                        e.matmul(ps[:, a * 32 + 8:a * 32 + 16], lhsT=cm[:, 1, :], rhs=tok[:, n, 56:64], start=True, stop=True)
                        e.matmul(ps[:, a * 32 + 16:a * 32 + 24], lhsT=ones_f[:, :], rhs=tok[:, n, 40:48], start=True, stop=True)
                        inst = e.matmul(ps[:, a * 32 + 24:a * 32 + 32], lhsT=ones_f[:, :], rhs=tok[:, n, 56:64], start=True, stop=True)
                    return inst
                sc.op("pe", fc, reads=[("tok", n0), ("tok", n0 + 8), "cm", "ones"], writes=[pk])
                sc.op("dve", lambda e, ps=ps, n0=n0, nn=nn: e.tensor_copy(
                    gcall[:, n0:n0 + nn, :], ps[:, 0:nn * 32].rearrange("p (a c) -> p a c", c=32)),
                    reads=[pk], writes=["gcall"])
            alltok = [("tok", n0) for n0 in range(0, NT, 8)]
            sc.op("dve", lambda e: e.tensor_copy(A_beta[:, :, 0:8], tok[:, :, 0:8]), reads=alltok, writes=["beta"])
            sc.op("dve", lambda e: e.tensor_copy(A_beta[:, :, 8:16], tok[:, :, 16:24]), reads=alltok + ["beta"], writes=["beta"])
            sc.op("dve", lambda e: e.tensor_scalar(out=A_nbeta[:, :, :], in0=A_beta[:, :, :], scalar1=-1.0, scalar2=None,
                                                    op0=ALU.mult), reads=["beta"], writes=["nbeta"])
            sc.op("dve", lambda e: e.tensor_copy(A_gc[:, :, :], gcall[:, :, 0:16]), reads=["gcall"], writes=["gc"])
            sc.op("dve", lambda e: e.tensor_scalar(out=A_ngc[:, :, :], in0=gcall[:, :, 0:16], scalar1=-1.0, scalar2=None,
                                                    op0=ALU.mult), reads=["gcall"], writes=["ngc"])
            sc.op("act", lambda e: e.activation(out=tmp[:, :, :], in_=gcall[:, :, 0:16], func=AF.Exp), reads=["gcall"], writes=["tmp"])
            sc.op("dve", lambda e: e.tensor_tensor(out=A_bg[:, :, :], in0=A_beta[:, :, :], in1=tmp[:, :, :], op=ALU.mult),
                  reads=["beta", "tmp"], writes=["bg"])
            sc.op("dve", lambda e: e.tensor_tensor(out=tmp[:, :, :], in0=gcall[:, :, 16:32], in1=gcall[:, :, 0:16], op=ALU.subtract),
                  reads=["gcall", "tmp", "bg"], writes=["tmp"])
            sc.op("act", lambda e: e.activation(out=A_ekd[:, :, :], in_=tmp[:, :, :], func=AF.Exp), reads=["tmp"], writes=["ekd"])
            sc.op("act", lambda e: e.activation(out=A_gl[:, :, :], in_=gcall[:, :, 16:32], func=AF.Exp), reads=["gcall"], writes=["gl"])
            sc.emit()

        import os
        if os.environ.get("DN_SKIP_DM"):
            return
        with ExitStack() as ctx:
            sc.begin(ctx, "dm")
            qT_all = sc.sb("qT", [128, S], BF16)
            kT_all = sc.sb("kT", [128, S], BF16)
            vT_all = sc.sb("vT", [128, 2, S], BF16)
            S_f = sc.sb("Sf", [128, 4, 128], F32)
            RD = F32
            S_b = sc.sb("Sb", [128, 4, 128], RD)
            psF = Ring([(sc.ps(f"psF{i}", [128, 512]), f"psF{i}") for i in range(5)])
            psKR = Ring([(sc.ps(f"psK{i}", [128, 512]), f"psK{i}") for i in range(2)])
            psT = Ring([(sc.ps(f"psT{i}", [128, 1024], BF16), f"psT{i}") for i in range(1)])

            def ring(name, n, shape, dt):
                return Ring([(sc.sb(f"{name}{i}", shape, dt), f"{name}{i}") for i in range(n)])
            R_tokv = ring("tokv", 4, [128, 3, 128], RD)
            R_dg = ring("dg", 6, [128, 128], F32)
            R_egr = ring("egr", 6, [128, 128], BF16)
            R_Dp = ring("Dp", 6, [128, 128], F32)
            R_Dn = ring("Dn", 6, [128, 128], F32)
            R_BM = ring("BM", 12, [128, 2, 128], F32)
            R_P = ring("P", 12, [128, 128], F32)
            R_TT = ring("TT", 6, [128, 128], RD)
            R_qk = ring("qk", 6, [128, 128], RD)
            R_qd = ring("qd", 6, [128, 128], RD)
            R_vb = ring("vb", 6, [128, 128], RD)
            R_kbg = ring("kbg", 6, [128, 128], RD)
            R_kd = ring("kd", 6, [128, 128], RD)
            R_wT = ring("wT", 6, [128, 128], RD)
            R_vn = ring("vn", 6, [128, 128], RD)
            R_o = ring("o", 6, [128, 128], F32)

            def unit(n, hk, j, d, tokv, tvk, psK, pkk):
                s16 = d * 8 + 2 * hk + j
                ci = j * 2 + d
                tsl = slice(n * 128, (n + 1) * 128)
                col = lambda A: A[:, n, s16:s16 + 1]
                skey = ("S", ci)
                dg, dgk = R_dg.next()
                sc.op("pool", lambda e: e.tensor_scalar(out=dg[:, :], in0=ident_f[:, :], scalar1=col(A_gc), scalar2=None,
                                                         op0=ALU.mult), reads=[], writes=[dgk])
                psR, prk = psF.next()

                def fr(e):
                    e.matmul(psR[:, 0:128], lhsT=ones_f[:, :], rhs=dg[:, :], start=True, stop=True)
                    e.matmul(psR[:, 128:256], lhsT=ones_f[:, :], rhs=dg[:, :], start=True, stop=False)
                    e.matmul(psR[:, 128:256], lhsT=ident_f[:, :], rhs=cm[:, 2 + 2 * d, :], start=False, stop=True)
                    e.matmul(psR[:, 256:384], lhsT=ones_f[:, :], rhs=dg[:, :], start=True, stop=False)
                    return e.matmul(psR[:, 256:384], lhsT=ident_f[:, :], rhs=cm[:, 3 + 2 * d, :], start=False, stop=True)
                sc.op("pe", fr, reads=[dgk], writes=[prk])
                yield
                egr, egk = R_egr.next()
                Dp, Dpk = R_Dp.next()
                Dn, Dnk = R_Dn.next()
                sc.op("act", lambda e: e.activation(out=egr[:, :], in_=psR[:, 0:128], func=AF.Exp), reads=[prk], writes=[egk])
                sc.op("act", lambda e: e.activation(out=Dp[:, :], in_=psR[:, 128:256], func=AF.Exp, scale=-1.0, bias=col(A_gc)),
                      reads=[prk], writes=[Dpk])
                sc.op("act", lambda e: e.activation(out=Dn[:, :], in_=psR[:, 256:384], func=AF.Exp, scale=1.0, bias=col(A_ngc)),
                      reads=[prk], writes=[Dnk])
                yield
                BM0, BM0k = R_BM.next()
                sc.op("dve", lambda e: e.scalar_tensor_tensor(out=BM0[:, 0, :], in0=psK[:, 0:128], scalar=col(A_nbeta), in1=Dp[:, :],
                                                               op0=ALU.mult, op1=ALU.mult), reads=[pkk, Dpk], writes=[BM0k])
                qk, qkk = R_qk.next()
                sc.op("dve", lambda e: e.tensor_tensor(out=qk[:, :], in0=psK[:, 128:256], in1=Dn[:, :], op=ALU.mult),
                      reads=[pkk, Dnk], writes=[qkk])
                qd, qdk = R_qd.next()
                sc.op("pool", lambda e: e.tensor_tensor(out=qd[:, :], in0=qT_all[:, tsl], in1=egr[:, :], op=ALU.mult),
                      reads=["qT", egk], writes=[qdk])
                vb, vbk = R_vb.next()
                kbg, kbgk = R_kbg.next()
                kd, kdk = R_kd.next()
                sc.op("pool", lambda e: e.tensor_scalar(out=vb[:, :], in0=tokv[:, 1 + j, :], scalar1=col(A_beta), scalar2=None,
                                                         op0=ALU.mult), reads=[tvk], writes=[vbk])
                sc.op("pool", lambda e: e.tensor_scalar(out=kbg[:, :], in0=tokv[:, 0, :], scalar1=col(A_bg), scalar2=None,
                                                         op0=ALU.mult), reads=[tvk], writes=[kbgk])
                sc.op("pool", lambda e: e.tensor_scalar(out=kd[:, :], in0=tokv[:, 0, :], scalar1=col(A_ekd), scalar2=None,
                                                         op0=ALU.mult), reads=[tvk], writes=[kdk])
                psM, pmk = psF.next()
                sc.op("pe", lambda e: e.transpose(psM[:, 0:128], BM0[:, 0, :], ident_f[:, :]), reads=[BM0k], writes=[pmk])
                yield
                P0, P0k = R_P.next()
                sc.op("act", lambda e: e.activation(out=BM0[:, 1, :], in_=psM[:, 0:128], func=AF.Copy), reads=[pmk, BM0k], writes=[BM0k])
                yield
                sc.op("dve", lambda e: e.tensor_tensor(out=P0[:, :], in0=BM0[:, 1, :], in1=ident_f[:, :], op=ALU.add),
                      reads=[BM0k], writes=[P0k])
                BM, BMk, P, Pk = BM0, BM0k, P0, P0k
                yield
                TT = TTk = None
                for k in range(6):
                    psD, pdk = psF.next()
                    last = (k == 5)

                    def fd(e, psD=psD, BM=BM, last=last):
                        inst = e.matmul(psD[:, 0:128], lhsT=BM[:, 1, :], rhs=BM[:, 0, :], start=True, stop=True)
                        if not last:
                            inst = e.matmul(psD[:, 128:256], lhsT=BM[:, 0, :], rhs=BM[:, 1, :], start=True, stop=True)
                        return inst
                    sc.op("pe", fd, reads=[BMk], writes=[pdk])
                    yield
                    BM2, BM2k = R_BM.next()
                    w2 = 128 if last else 256
                    evac_eng = "act" if k % 2 == 0 else "dve"
                    if evac_eng == "act":
                        sc.op("act", lambda e, BM2=BM2, psD=psD, w2=w2: e.activation(
                            out=BM2[:, :, :].rearrange("p a c -> p (a c)")[:, 0:w2], in_=psD[:, 0:w2], func=AF.Copy),
                            reads=[pdk], writes=[BM2k])
                    else:
                        sc.op("dve", lambda e, BM2=BM2, psD=psD, w2=w2: e.tensor_copy(
                            BM2[:, :, :].rearrange("p a c -> p (a c)")[:, 0:w2], psD[:, 0:w2]),
                            reads=[pdk], writes=[BM2k])
                    yield
                    psE, pek = psF.next()
                    sc.op("pe", lambda e, psE=psE, BM2=BM2, P=P: e.matmul(psE[:, 0:128], lhsT=BM2[:, 0, :], rhs=P[:, :], start=True, stop=True),
                          reads=[BM2k, Pk], writes=[pek])
                    yield
                    if not last:
                        P2, P2k = R_P.next()
                        sc.op("dve", lambda e, P2=P2, P=P, psE=psE: e.tensor_tensor(out=P2[:, :], in0=psE[:, 0:128], in1=P[:, :], op=ALU.add),
                              reads=[pek, Pk], writes=[P2k])
                        P, Pk = P2, P2k
                        BM, BMk = BM2, BM2k
                    else:
                        TT, TTk = R_TT.next()
                        sc.op("dve", lambda e, TT=TT, P=P, psE=psE: e.tensor_tensor(out=TT[:, :], in0=psE[:, 0:128], in1=P[:, :], op=ALU.add),
                              reads=[pek, Pk], writes=[TTk])
                    yield
                psW, pwk = psF.next()
                sc.op("pe", lambda e: e.matmul(psW[:, 0:128], lhsT=kbg[:, :], rhs=TT[:, :], start=True, stop=True),
                      reads=[kbgk, TTk], writes=[pwk])
                yield
                wT, wTk = R_wT.next()
                sc.op("act", lambda e: e.activation(out=wT[:, :], in_=psW[:, 0:128], func=AF.Copy, scale=-1.0), reads=[pwk], writes=[wTk])
                yield
                psV, pvk = psF.next()

                def fv(e):
                    e.matmul(psV[:, 0:128], lhsT=TT[:, :], rhs=vb[:, :], start=True, stop=False)
                    return e.matmul(psV[:, 0:128], lhsT=wT[:, :], rhs=S_b[:, ci, :], start=False, stop=True)
                sc.op("pe", fv, reads=[TTk, vbk, wTk, skey], writes=[pvk])
                yield
                vn, vnk = R_vn.next()
                sc.op("act", lambda e: e.activation(out=vn[:, :], in_=psV[:, 0:128], func=AF.Copy), reads=[pvk], writes=[vnk])
                yield
                psO, pok = psF.next()

                def fo(e):
                    e.matmul(psO[:, 0:128], lhsT=S_b[:, ci, :], rhs=qd[:, :], start=True, stop=False)
                    e.matmul(psO[:, 0:128], lhsT=vn[:, :], rhs=qk[:, :], start=False, stop=True)
                    return e.matmul(psO[:, 128:256], lhsT=kd[:, :], rhs=vn[:, :], start=True, stop=True)
                sc.op("pe", fo, reads=[skey, qdk, vnk, qkk, kdk], writes=[pok])
                yield
                ot, otk = R_o.next()
                sc.op("dve", lambda e: e.tensor_copy(ot[:, :], psO[:, 0:128]), reads=[pok], writes=[otk])
                sc.dma("sp", dn_o[2 * hk + j, d][:, tsl], ot[:, :], reads=[otk], writes=[("dno", ci)])
                sc.op("dve", lambda e: e.scalar_tensor_tensor(out=S_f[:, ci, :], in0=S_f[:, ci, :], scalar=col(A_gl), in1=psO[:, 128:256],
                                                               op0=ALU.mult, op1=ALU.add), reads=[pok, ("Sf", ci), skey], writes=[("Sf", ci)])
                sc.op("act", lambda e: e.activation(out=S_b[:, ci, :], in_=S_f[:, ci, :], func=AF.Copy), reads=[("Sf", ci)], writes=[skey])
                yield

            for hk in hk_list:
                sc.dma("sp", qT_all[:, :], dn_q[hk], writes=["qT"])
                sc.dma("sp", kT_all[:, :], dn_k[hk], writes=["kT"])
                sc.dma("sp", vT_all[:, 0, :], dn_v[2 * hk], writes=["vT"])
                sc.dma("sp", vT_all[:, 1, :], dn_v[2 * hk + 1], reads=["vT"], writes=["vT"])
                for ci in range(4):
                    sc.op("pool", lambda e, ci=ci: e.memset(S_f[:, ci, :], 0.0), writes=[("Sf", ci)])
                    sc.op("pool", lambda e, ci=ci: e.memset(S_b[:, ci, :], 0.0), writes=[("S", ci)])
                for s in range(NT):
                    gens = []
                    for d in (0, 1):
                        n = s if d == 0 else NT - 1 - s
                        tsl = slice(n * 128, (n + 1) * 128)
                        pst, ptk = psT.next()

                        def ftr(e, pst=pst, tsl=tsl):
                            e.transpose(pst[:, 0:128], kT_all[:, tsl], ident_b[:, :])
                            e.transpose(pst[:, 128:256], vT_all[:, 0, tsl], ident_b[:, :])
                            return e.transpose(pst[:, 256:384], vT_all[:, 1, tsl], ident_b[:, :])
                        sc.op("pe", ftr, reads=["kT", "vT"], writes=[ptk])
                        tokv, tvk = R_tokv.next()
                        sc.op("act", lambda e, tokv=tokv, pst=pst: e.activation(
                            out=tokv[:, :, :], in_=pst[:, 0:384].rearrange("p (a c) -> p a c", a=3), func=AF.Copy),
                            reads=[ptk], writes=[tvk])
                        psK, pkk = psKR.next()

                        def fk(e, psK=psK, tsl=tsl):
                            e.matmul(psK[:, 0:128], lhsT=kT_all[:, tsl], rhs=kT_all[:, tsl], start=True, stop=True)
                            return e.matmul(psK[:, 128:256], lhsT=kT_all[:, tsl], rhs=qT_all[:, tsl], start=True, stop=True)
                        sc.op("pe", fk, reads=["kT", "qT"], writes=[pkk])
                        for j in (0, 1):
                            gens.append(unit(n, hk, j, d, tokv, tvk, psK, pkk))
                    alive = list(gens)
                    lim = int(os.environ.get("DN_STOP", "1000"))
                    it = 0
                    while alive and it < lim:
                        it += 1
                        nxt = []
                        for gto in alive:
                            try:
                                next(gto)
                                nxt.append(gto)
                            except StopIteration:
                                pass
                        alive = nxt
            sc.emit()


def make_cmat():
    i = np.arange(128)[:, None]
    j = np.arange(128)[None, :]
    tri_f = (i <= j).astype(np.float32)
    tri_b = (i >= j).astype(np.float32)
    P_f = np.where(j >= i, BIG, 0.0).astype(np.float32)
    N_f = np.where(j < i, -BIG, 0.0).astype(np.float32)
    P_b = np.where(j <= i, BIG, 0.0).astype(np.float32)
    N_b = np.where(j > i, -BIG, 0.0).astype(np.float32)
    return np.stack([tri_f, tri_b, P_f, N_f, P_b, N_b])


def phase_dn_final(sc, nc, S, dn_o, dn_zg, normw, o_bT, n_hv=8):
    with ExitStack() as ctx:
        sc.begin(ctx, "df")
        nw = sc.sb("nw", [128, 1], F32)
        ones_f = sc.sb("ones", [128, 128], F32)
        ofR = Ring([(sc.sb(f"of{i}", [128, 512], F32), f"of{i}") for i in range(2)])
        obR = Ring([(sc.sb(f"ob{i}", [128, 512], F32), f"ob{i}") for i in range(2)])
        zgR = Ring([(sc.sb(f"zg{i}", [128, 512], BF16), f"zg{i}") for i in range(2)])
        sqR = Ring([(sc.sb(f"sq{i}", [128, 512], F32), f"sq{i}") for i in range(2)])
        rsR = Ring([(sc.sb(f"rs{i}", [128, 512], F32), f"rs{i}") for i in range(2)])
        psR = Ring([(sc.ps(f"ps{i}", [128, 512]), f"ps{i}") for i in range(2)])
        sc.dma("sp", nw[:, :], normw, writes=["nw"])
        sc.op("pool", lambda e: e.memset(ones_f[:, :], 1.0), writes=["ones"])
        epsc = sc.sb("epsc", [128, 1], F32)
        sc.op("pool", lambda e: e.memset(epsc[:, :], 128.0 * 1e-6), writes=["eps"])
        sc.op("dve", lambda e: e.tensor_scalar(out=nw[:, :], in0=nw[:, :], scalar1=float(HEAD_DIM) ** 0.5, scalar2=None,
                                                op0=ALU.mult), reads=["nw"], writes=["nw"])
        for hv in range(n_hv):
            for c0 in range(0, S, 512):
                sl = slice(c0, c0 + 512)
                of, ofk = ofR.next()
                ob, obk = obR.next()
                zg, zgk = zgR.next()
                sc.dma("sp", of[:, :], dn_o[hv, 0][:, sl], writes=[ofk])
                sc.dma("sp", ob[:, :], dn_o[hv, 1][:, sl], writes=[obk])
                sc.dma("sp", zg[:, :], dn_zg[hv][:, sl], writes=[zgk])
                sc.op("dve", lambda e, of=of, ob=ob: e.tensor_tensor(out=of[:, :], in0=of[:, :], in1=ob[:, :], op=ALU.add),
                      reads=[ofk, obk], writes=[ofk])
                sq, sqk = sqR.next()
                sc.op("act", lambda e, sq=sq, of=of: e.activation(out=sq[:, :], in_=of[:, :], func=AF.Square), reads=[ofk], writes=[sqk])
                ps, pk = psR.next()
                sc.op("pe", lambda e, ps=ps, sq=sq: e.matmul(ps[:, :], lhsT=ones_f[:, :], rhs=sq[:, :], start=True, stop=True),
                      reads=[sqk, "ones"], writes=[pk])
                rs, rsk = rsR.next()
                sc.op("act", lambda e, rs=rs, ps=ps: e.activation(out=rs[:, :], in_=ps[:, :], func=AF.Ln, bias=epsc[:, 0:1]),
                      reads=[pk, "eps"], writes=[rsk])
                sc.op("act", lambda e, rs=rs: e.activation(out=rs[:, :], in_=rs[:, :], func=AF.Exp, scale=-0.5),
                      reads=[rsk], writes=[rsk])
                sc.op("dve", lambda e, rs=rs, of=of: e.tensor_tensor(out=of[:, :], in0=of[:, :], in1=rs[:, :], op=ALU.mult),
                      reads=[ofk, rsk], writes=[ofk])
                sc.op("dve", lambda e, of=of, zg=zg: e.scalar_tensor_tensor(out=of[:, :], in0=of[:, :], scalar=nw[:, 0:1], in1=zg[:, :],
                                                                             op0=ALU.mult, op1=ALU.mult), reads=[ofk, zgk, "nw"], writes=[ofk])
                sc.dma("sp", o_bT[hv * 128:(hv + 1) * 128, sl], of[:, :], reads=[ofk], writes=[("obT", hv, c0)])
        sc.emit()


N_EXPERTS = 32
ALPHA = 2.0 ** 0.25
LN_EPS = 1e-5


def _layernorm(sc, r, rk, n2048, gam, bet, scratch, tag, out_t, outk):
    st = scratch
    sc.op("dve", lambda e: e.reduce_sum(out=st["sum"][:, 0:1], in_=r, axis=AX.X), reads=[rk], writes=[tag + "sum"])
    sc.op("act", lambda e: e.activation(out=st["sq"][:, :], in_=r, func=AF.Square), reads=[rk], writes=[tag + "sq"])
    sc.op("dve", lambda e: e.reduce_sum(out=st["sum"][:, 1:2], in_=st["sq"][:, :], axis=AX.X),
          reads=[tag + "sq", tag + "sum"], writes=[tag + "sum"])
    sc.op("dve", lambda e: e.tensor_scalar(out=st["sum"][:, 0:2], in0=st["sum"][:, 0:2], scalar1=1.0 / n2048, scalar2=None,
                                            op0=ALU.mult), reads=[tag + "sum"], writes=[tag + "sum"])
    sc.op("dve", lambda e: e.tensor_tensor(out=st["sum"][:, 2:3], in0=st["sum"][:, 0:1], in1=st["sum"][:, 0:1], op=ALU.mult),
          reads=[tag + "sum"], writes=[tag + "sum"])
    sc.op("dve", lambda e: e.tensor_tensor(out=st["sum"][:, 1:2], in0=st["sum"][:, 1:2], in1=st["sum"][:, 2:3], op=ALU.subtract),
          reads=[tag + "sum"], writes=[tag + "sum"])
    sc.op("act", lambda e: e.activation(out=st["sum"][:, 1:2], in_=st["sum"][:, 1:2], func=AF.Ln, bias=st["eps"][:, 0:1]),
          reads=[tag + "sum", "lneps"], writes=[tag + "sum"])
    sc.op("act", lambda e: e.activation(out=st["sum"][:, 1:2], in_=st["sum"][:, 1:2], func=AF.Exp, scale=-0.5),
          reads=[tag + "sum"], writes=[tag + "sum"])
    sc.op("dve", lambda e: e.tensor_scalar(out=r, in0=r, scalar1=st["sum"][:, 0:1], scalar2=st["sum"][:, 1:2],
                                            op0=ALU.subtract, op1=ALU.mult), reads=[rk, tag + "sum"], writes=[rk])
    sc.op("pool", lambda e: e.tensor_tensor(out=r, in0=r, in1=gam[:, :], op=ALU.mult), reads=[rk, "lnconst"], writes=[rk])
    sc.op("pool", lambda e: e.tensor_tensor(out=out_t, in0=r, in1=bet[:, :], op=ALU.add), reads=[rk, "lnconst"], writes=[outk])


def phase_tail1a(sc, nc, T, d):
    NTT = T // 512
    with ExitStack() as ctx:
        sc.begin(ctx, "ta")
        bgate = sc.sb("bgate", [128, 32], F32)
        ln_g = sc.sb("lng", [128, 2048], F32)
        ln_b = sc.sb("lnb", [128, 2048], F32)
        lneps = sc.sb("lneps", [128, 1], F32)
        xb = sc.sb("xb", [128, KC, 512], BF16)
        oab = sc.sb("oab", [128, 24, 512], BF16)
        gates = sc.sb("gates", [128, 16, 512], BF16)
        mb = sc.sb("mb", [128, KC, 512], BF16)
        wR = Ring([(sc.sb(f"w{i}", [128, KC, 512], BF16), f"w{i}") for i in range(2)])
        tmpR = Ring([(sc.sb(f"tmp{i}", [128, 512], F32), f"tmp{i}") for i in range(2)])
        r1R = Ring([(sc.sb(f"r1{i}", [128, 2048], F32), f"r1{i}") for i in range(2)])
        xtR = Ring([(sc.sb(f"xt{i}", [128, 512], F32), f"xt{i}") for i in range(3)])
        lnst = {"sum": sc.sb("lnsum", [128, 4], F32), "sq": sc.sb("lnsq", [128, 2048], BF16), "eps": lneps}
        psP = Ring([(sc.ps(f"psP{i}", [128, 512]), f"psP{i}") for i in range(4)])
        psQ = Ring([(sc.ps(f"psQ{i}", [128, 512]), f"psQ{i}") for i in range(3)])
        sc.dma("sp", bgate[:, :], d["bgate"], writes=["bgate"])
        sc.dma("sp", ln_g[:, :], d["ln1g"], writes=["lnconst"])
        sc.dma("sp", ln_b[:, :], d["ln1b"], writes=["lnconst"])
        sc.op("pool", lambda e: e.memset(lneps[:, :], LN_EPS), writes=["lneps"])

        def wblocks(W, kcn, c0, ncols):
            for blk in range(ncols // 512):
                w, wk = wR.next()
                sc.dma("pool", w[:, 0:kcn, :], W[:, c0 + blk * 512:c0 + (blk + 1) * 512].rearrange("(kc p) n -> p kc n", p=128),
                       writes=[wk])
                yield blk, w, wk

        for tt in range(NTT):
            tsl = slice(tt * 512, (tt + 1) * 512)
            sc.dma("pool", xb[:, :, :], d["xT"][:, tsl].rearrange("(kc p) t -> p kc t", p=128), writes=["xb"])
            sc.dma("pool", oab[:, :, :], d["oT"][:, tsl].rearrange("(kc p) t -> p kc t", p=128), writes=["oab"])
            for br in range(2):
                for blk, w, wk in wblocks(d["wg"], KC, br * 2048, 2048):
                    for jj in range(4):
                        j = blk * 4 + jj
                        ps, pk = psP.next()

                        def f(e, ps=ps, w=w, jj=jj):
                            for kc in range(KC):
                                inst = e.matmul(ps[:, :], lhsT=w[:, kc, jj * 128:(jj + 1) * 128], rhs=xb[:, kc, :],
                                                start=(kc == 0), stop=(kc == KC - 1))
                            return inst
                        sc.op("pe", f, reads=[wk, "xb"], writes=[pk])
                        sc.op("act", lambda e, ps=ps, j=j, br=br: e.activation(out=gates[:, j, :], in_=ps[:, :], func=AF.Sigmoid,
                                                                                bias=bgate[:, br * 16 + j:br * 16 + j + 1]),
                              reads=[pk, "bgate"], writes=[("gates", j)])
                kcn = 8 if br == 0 else KC
                koff = 0 if br == 0 else 8
                for blk, w, wk in wblocks(d["wa"] if br == 0 else d["wb"], kcn, 0, 2048):
                    for jj in range(4):
                        j = blk * 4 + jj
                        ps, pk = psP.next()

                        def f(e, ps=ps, w=w, jj=jj, kcn=kcn, koff=koff):
                            for kc in range(kcn):
                                inst = e.matmul(ps[:, :], lhsT=w[:, kc, jj * 128:(jj + 1) * 128], rhs=oab[:, koff + kc, :],
                                                start=(kc == 0), stop=(kc == kcn - 1))
                            return inst
                        sc.op("pe", f, reads=[wk, "oab"], writes=[pk])
                        if br == 0:
                            sc.op("dve", lambda e, ps=ps, j=j: e.tensor_tensor(out=mb[:, j, :], in0=ps[:, :], in1=gates[:, j, :], op=ALU.mult),
                                  reads=[pk, ("gates", j)], writes=[("mb", j)])
                        else:
                            tp, tk = tmpR.next()
                            sc.op("dve", lambda e, ps=ps, j=j, tp=tp: e.tensor_tensor(out=tp[:, :], in0=ps[:, :], in1=gates[:, j, :], op=ALU.mult),
                                  reads=[pk, ("gates", j)], writes=[tk])
                            sc.op("pool", lambda e, j=j, tp=tp: e.tensor_tensor(out=mb[:, j, :], in0=mb[:, j, :], in1=tp[:, :], op=ALU.add),
                                  reads=[tk, ("mb", j)], writes=[("mb", j)])
            mbk = [("mb", j) for j in range(KC)]
            for half in range(2):
                subs = (2 * half, 2 * half + 1)
                bufs = {}
                for sub in subs:
                    bufs[sub] = r1R.next()
                for blk, w, wk in wblocks(d["wout"], KC, 0, 2048):
                    for sub in subs:
                        r1, r1k = bufs[sub]
                        n128 = tt * 4 + sub
                        xt, xtk = xtR.next()
                        sc.dma("sp", xt[:, :], d["x"][n128 * 128:(n128 + 1) * 128, blk * 512:(blk + 1) * 512], writes=[xtk])
                        ps, pk = psQ.next()

                        def f(e, ps=ps, w=w, sub=sub):
                            for kc in range(KC):
                                inst = e.matmul(ps[:, :], lhsT=mb[:, kc, sub * 128:(sub + 1) * 128], rhs=w[:, kc, :],
                                                start=(kc == 0), stop=(kc == KC - 1))
                            return inst
                        sc.op("pe", f, reads=[wk] + mbk, writes=[pk])
                        sc.op("dve", lambda e, ps=ps, r1=r1, xt=xt, blk=blk: e.scalar_tensor_tensor(
                            out=r1[:, blk * 512:(blk + 1) * 512], in0=xt[:, :], scalar=ALPHA,
                            in1=ps[:, :], op0=ALU.mult, op1=ALU.add), reads=[pk, xtk, r1k], writes=[r1k])
                for sub in subs:
                    r1, r1k = bufs[sub]
                    n128 = tt * 4 + sub
                    _layernorm(sc, r1[:, :], r1k, 2048.0, ln_g, ln_b, lnst, "ln1", r1[:, :], r1k)
                    sc.dma("sp", d["h_scr"][n128 * 128:(n128 + 1) * 128, :], r1[:, :], reads=[r1k], writes=[("hscr", n128)])
        sc.emit()


def phase_tail1b(sc, nc, T, C, d):
    NTT = T // 512
    with ExitStack() as ctx:
        sc.begin(ctx, "tb")
        ident_f = sc.sb("identf", [128, 128], F32)
        ones_f = sc.sb("onesf", [128, 128], F32)
        UT = sc.sb("UT", [128, 128], F32)
        bpg = sc.sb("bpg", [128, 2048], F32)
        wr = sc.sb("wr", [128, KC, 32], F32)
        rb = sc.sb("rb", [128, 32], F32)
        ebase = sc.sb("ebase", [128, 32], F32)
        cumsel = sc.sb("cumsel", [128, 32], F32)
        pos_i = sc.sb("posi", [128, T // 128, 4], I32)
        gate4 = sc.sb("gate4", [128, T // 128, 4], F32)
        pTb = sc.sb("pTb", [128, 2, 512], BF16)
        hTf = sc.sb("hTf", [128, KC, 128], F32)
        hTb = sc.sb("hTb", [128, KC, 512], BF16)
        pgs = [sc.sb(f"pg{i}", [128, 2048], F32) for i in range(4)]
        wR = Ring([(sc.sb(f"w{i}", [128, KC, 512], BF16), f"w{i}") for i in range(2)])
        hR = Ring([(sc.sb(f"h{i}", [128, 2048], F32), f"h{i}") for i in range(2)])
        hbR = Ring([(sc.sb(f"hb{i}", [128, 2048], BF16), f"hb{i}") for i in range(2)])
        lg = sc.sb("lg", [128, 32], F32)
        m8 = sc.sb("m8", [128, 8], F32)
        sel = sc.sb("sel", [128, 32], F32)
        oh = sc.sb("oh", [128, 32], F32)
        posf = sc.sb("posf", [128, 32], F32)
        pf4 = sc.sb("pf4", [128, 4], F32)
        e4 = sc.sb("e4", [128, 4], F32)
        s1 = sc.sb("s1", [128, 2], F32)
        psQ = Ring([(sc.ps(f"psQ{i}", [128, 512]), f"psQ{i}") for i in range(6)])
        ixR = Ring([(sc.sb(f"ix{i}", [128, 1], I32), f"ix{i}") for i in range(8)])
        sc.dma("sp", ident_f[:, :], d["ident"], writes=["ident"])
        sc.dma("sp", UT[:, :], d["UT"], writes=["UT"])
        sc.dma("sp", bpg[:, :], d["bpg"], writes=["bpg"])
        sc.dma("sp", wr[:, :, :], d["wr"].rearrange("(kc p) n -> p kc n", p=128), writes=["wr"])
        sc.dma("sp", rb[:, :], d["rb"], writes=["rb"])
        sc.dma("sp", ebase[:, :], d["ebase"], writes=["ebase"])
        sc.op("pool", lambda e: e.memset(ones_f[:, :], 1.0), writes=["ones"])
        sc.op("pool", lambda e: e.memset(cumsel[:, :], 0.0), writes=["cumsel"])

        def wblocks(W, kcn, ncols):
            for blk in range(ncols // 512):
                w, wk = wR.next()
                sc.dma("pool", w[:, 0:kcn, :], W[:, blk * 512:(blk + 1) * 512].rearrange("(kc p) n -> p kc n", p=128),
                       writes=[wk])
                yield blk, w, wk

        for tt in range(NTT):
            tsl = slice(tt * 512, (tt + 1) * 512)
            sc.dma("pool", pTb[:, :, :], d["pT"][:, tsl].rearrange("(kc p) t -> p kc t", p=128), writes=["pTb"])
            for sub in range(4):
                n128 = tt * 4 + sub
                h, hk = hR.next()
                sc.dma("sp", h[:, :], d["h_scr"][n128 * 128:(n128 + 1) * 128, :], writes=[hk])
                hb, hbk = hbR.next()
                sc.op("act", lambda e, hb=hb, h=h: e.activation(out=hb[:, :], in_=h[:, :], func=AF.Copy), reads=[hk], writes=[hbk])
                for k0 in range(0, KC, 4):
                    ps, pk = psQ.next()

                    def ft(e, ps=ps, h=h, k0=k0):
                        for a in range(4):
                            inst = e.transpose(ps[:, a * 128:(a + 1) * 128], h[:, (k0 + a) * 128:(k0 + a + 1) * 128], ident_f[:, :])
                        return inst
                    sc.op("pe", ft, reads=[hk, "ident"], writes=[pk])
                    sc.op("act", lambda e, ps=ps, k0=k0: e.activation(
                        out=hTf[:, k0:k0 + 4, :], in_=ps[:, :].rearrange("p (a t) -> p a t", a=4), func=AF.Copy),
                        reads=[pk], writes=["hTf"])
                sc.op("pool", lambda e, sub=sub: e.tensor_copy(hTb[:, :, sub * 128:(sub + 1) * 128], hTf[:, :, :]),
                      reads=["hTf"], writes=[("hTb", sub)])
                ps, pk = psQ.next()

                def fr(e, ps=ps):
                    for kc in range(KC):
                        inst = e.matmul(ps[:, 0:32], lhsT=hTf[:, kc, :], rhs=wr[:, kc, :], start=(kc == 0), stop=(kc == KC - 1))
                    return inst
                sc.op("pe", fr, reads=["hTf", "wr"], writes=[pk])
                R = ["rt"]
                sc.op("dve", lambda e, ps=ps: e.tensor_tensor(out=lg[:, :], in0=ps[:, 0:32], in1=rb[:, :], op=ALU.add),
                      reads=[pk, "rb"] + R, writes=R)
                sc.op("dve", lambda e: e.max(m8[:, :], lg[:, :]), reads=R, writes=R)
                sc.op("dve", lambda e: e.tensor_scalar(out=sel[:, :], in0=lg[:, :], scalar1=m8[:, 3:4], scalar2=None, op0=ALU.is_ge),
                      reads=R, writes=R)
                sc.op("dve", lambda e: e.tensor_scalar(out=e4[:, :], in0=m8[:, 0:4], scalar1=m8[:, 0:1], scalar2=None, op0=ALU.subtract),
                      reads=R, writes=R)
                sc.op("act", lambda e: e.activation(out=e4[:, :], in_=e4[:, :], func=AF.Exp), reads=R, writes=R)
                sc.op("dve", lambda e: e.reduce_sum(out=s1[:, 0:1], in_=e4[:, :], axis=AX.X), reads=R, writes=R)
                sc.op("dve", lambda e: e.reciprocal(s1[:, 0:1], s1[:, 0:1]), reads=R, writes=R)
                sc.op("dve", lambda e, n128=n128: e.tensor_scalar(out=gate4[:, n128, :], in0=e4[:, :], scalar1=s1[:, 0:1], scalar2=None,
                                                                   op0=ALU.mult), reads=R, writes=R + ["gate4"])
                ps2, pk2 = psQ.next()

                def frank(e, ps2=ps2):
                    e.matmul(ps2[:, 0:32], lhsT=UT[:, :], rhs=sel[:, :], start=True, stop=False)
                    return e.matmul(ps2[:, 0:32], lhsT=ones_f[:, :], rhs=cumsel[:, :], start=False, stop=True)
                sc.op("pe", frank, reads=R + ["UT", "ones", "cumsel"], writes=[pk2])
                sc.op("dve", lambda e, ps2=ps2: e.tensor_scalar(out=posf[:, :], in0=ps2[:, 0:32], scalar1=float(C - 1), scalar2=None,
                                                                op0=ALU.min), reads=[pk2] + R, writes=R)
                sc.op("dve", lambda e: e.tensor_tensor(out=posf[:, :], in0=posf[:, :], in1=ebase[:, :], op=ALU.add),
                      reads=R + ["ebase"], writes=R)
                sc.op("pool", lambda e: e.tensor_tensor(out=cumsel[:, :], in0=cumsel[:, :], in1=sel[:, :], op=ALU.add),
                      reads=R + ["cumsel", pk2], writes=["cumsel"])
                for k in range(4):
                    sc.op("dve", lambda e, k=k: e.tensor_scalar(out=oh[:, :], in0=lg[:, :], scalar1=m8[:, k:k + 1], scalar2=None,
                                                                 op0=ALU.is_equal), reads=R, writes=R)
                    sc.op("dve", lambda e: e.tensor_tensor(out=oh[:, :], in0=oh[:, :], in1=posf[:, :], op=ALU.mult), reads=R, writes=R)
                    sc.op("dve", lambda e, k=k: e.reduce_sum(out=pf4[:, k:k + 1], in_=oh[:, :], axis=AX.X), reads=R, writes=R)
                sc.op("dve", lambda e, n128=n128: e.tensor_copy(pos_i[:, n128, :], pf4[:, :]), reads=R, writes=R + ["posi"])
                for k in range(4):
                    ix, ixk = ixR.next()
                    sc.op("dve", lambda e, ix=ix, k=k: e.tensor_copy(ix[:, :], pf4[:, k:k + 1]), reads=R, writes=[ixk])
                    sc.op("pool", lambda e, ix=ix, hb=hb: e.indirect_dma_start(
                        out=d["Xs"][:, :], out_offset=bass.IndirectOffsetOnAxis(ap=ix[:, :], axis=0),
                        in_=hb[:, :], in_offset=None),
                        reads=[hbk, ixk], writes=[("Xs", n128, k)], dma=True)
            for blk, w, wk in wblocks(d["wpg"], KC, 2048):
                for sub in range(4):
                    ps, pk = psQ.next()

                    def f(e, ps=ps, w=w, sub=sub):
                        for kc in range(KC):
                            inst = e.matmul(ps[:, :], lhsT=hTb[:, kc, sub * 128:(sub + 1) * 128], rhs=w[:, kc, :],
                                            start=(kc == 0), stop=(kc == KC - 1))
                        return inst
                    sc.op("pe", f, reads=[wk, ("hTb", sub)], writes=[pk])
                    sc.op("dve", lambda e, ps=ps, sub=sub, blk=blk: e.tensor_tensor(
                        out=pgs[sub][:, blk * 512:(blk + 1) * 512], in0=ps[:, :], in1=bpg[:, blk * 512:(blk + 1) * 512], op=ALU.add),
                        reads=[pk, "bpg", ("pg", sub)], writes=[("pg", sub)])
            for sub in range(4):
                sc.op("act", lambda e, sub=sub: e.activation(out=pgs[sub][:, :], in_=pgs[sub][:, :], func=AF.Sigmoid),
                      reads=[("pg", sub)], writes=[("pg", sub)])
            for blk, w, wk in wblocks(d["wple"], 2, 2048):
                for sub in range(4):
                    ps, pk = psQ.next()

                    def f(e, ps=ps, w=w, sub=sub):
                        for kc in range(2):
                            inst = e.matmul(ps[:, :], lhsT=pTb[:, kc, sub * 128:(sub + 1) * 128], rhs=w[:, kc, :],
                                            start=(kc == 0), stop=(kc == 1))
                        return inst
                    sc.op("pe", f, reads=[wk, "pTb"], writes=[pk])
                    sc.op("dve", lambda e, ps=ps, sub=sub, blk=blk: e.tensor_tensor(
                        out=pgs[sub][:, blk * 512:(blk + 1) * 512], in0=ps[:, :], in1=pgs[sub][:, blk * 512:(blk + 1) * 512], op=ALU.mult),
                        reads=[pk, ("pg", sub)], writes=[("pg", sub)])
            for sub in range(4):
                n128 = tt * 4 + sub
                h, hk = hR.next()
                sc.dma("sp", h[:, :], d["h_scr"][n128 * 128:(n128 + 1) * 128, :], writes=[hk])
                sc.op("dve", lambda e, sub=sub, h=h: e.scalar_tensor_tensor(out=pgs[sub][:, :], in0=h[:, :], scalar=ALPHA, in1=pgs[sub][:, :],
                                                                             op0=ALU.mult, op1=ALU.add), reads=[hk, ("pg", sub)], writes=[("pg", sub)])
                sc.dma("sp", d["r2_scr"][n128 * 128:(n128 + 1) * 128, :], pgs[sub][:, :], reads=[("pg", sub)], writes=[("r2scr", n128)])
        sc.dma("sp", d["pos_scr"], pos_i[:, :, :], reads=["posi"], writes=["pos_scr"])
        sc.dma("sp", d["gate_scr"], gate4[:, :, :], reads=["gate4"], writes=["gate_scr"])
        sc.emit()


def phase_experts(sc, nc, C, d, n_exp=N_EXPERTS):
    NSG = C // 512 if C >= 512 else 1
    SGW = min(C, 512)
    NST = C // 128
    with ExitStack() as ctx:
        sc.begin(ctx, "ex")
        ident_b = sc.sb("identb", [128, 128], BF16)
        ones_f = sc.sb("onesf", [1, 128], F32)
        xeT = sc.sb("xeT", [128, KC, C], BF16)
        actT = sc.sb("actT", [128, KC, C], BF16)
        bgR = Ring([(sc.sb(f"bg{i}", [128, 32], F32), f"bg{i}") for i in range(2)])
        bdR = Ring([(sc.sb(f"bd{i}", [1, 2048], F32), f"bd{i}") for i in range(2)])
        xeR = Ring([(sc.sb(f"xe{i}", [128, 2048], BF16), f"xe{i}") for i in range(2)])
        wR = Ring([(sc.sb(f"w{i}", [128, KC, 512], BF16), f"w{i}") for i in range(2)])
        gR = Ring([(sc.sb(f"g{i}", [128, SGW], F32), f"g{i}") for i in range(2)])
        uR = Ring([(sc.sb(f"u{i}", [128, SGW], F32), f"u{i}") for i in range(2)])
        sR = Ring([(sc.sb(f"s{i}", [128, SGW], F32), f"s{i}") for i in range(2)])
        yR = Ring([(sc.sb(f"y{i}", [128, 512], F32), f"y{i}") for i in range(3)])
        psT = Ring([(sc.ps("psT0", [128, 1024], BF16), "psT0")])
        psG = Ring([(sc.ps(f"psG{i}", [128, 512]), f"psG{i}") for i in range(4)])
        psY = Ring([(sc.ps(f"psY{i}", [128, 512]), f"psY{i}") for i in range(2)])
        sc.dma("pool", ident_b[:, :], d["ident"], writes=["identb"])
        sc.op("pool", lambda e: e.memset(ones_f[:, :], 1.0), writes=["ones"])
        for ex in range(n_exp):
            bg, bgk = bgR.next()
            sc.dma("sp", bg[:, :], d["bgu"][ex], writes=[bgk])
            bd, bdk = bdR.next()
            sc.dma("sp", bd[:, :], d["bd"][ex], writes=[bdk])
            for st in range(NST):
                xe, xek = xeR.next()
                sc.dma("sp", xe[:, :], d["Xs"][ex * C + st * 128:ex * C + (st + 1) * 128, :], writes=[xek])
                for k0 in range(0, KC, 8):
                    pt, ptk = psT.next()

                    def ft(e, pt=pt, xe=xe, k0=k0):
                        for a in range(8):
                            inst = e.transpose(pt[:, a * 128:(a + 1) * 128], xe[:, (k0 + a) * 128:(k0 + a + 1) * 128], ident_b[:, :])
                        return inst
                    sc.op("pe", ft, reads=[xek, "identb"], writes=[ptk])
                    sc.op("act", lambda e, pt=pt, k0=k0, st=st: e.activation(
                        out=xeT[:, k0:k0 + 8, st * 128:(st + 1) * 128], in_=pt[:, :].rearrange("p (a t) -> p a t", a=8), func=AF.Copy),
                        reads=[ptk], writes=[("xeT", st)])
            xeTk = [("xeT", st) for st in range(NST)]
            for blk in range(8):
                w, wk = wR.next()
                sc.dma("pool", w[:, :, :], d["wgu"][ex][:, blk * 512:(blk + 1) * 512].rearrange("(kc p) n -> p kc n", p=128), writes=[wk])
                for half in range(2):
                    fc = blk * 2 + half
                    for sg in range(NSG):
                        ssl = slice(sg * SGW, (sg + 1) * SGW)
                        psg, pgk = psG.next()
                        psu, puk = psG.next()

                        def fg(e, ps=psg, w=w, half=half, ssl=ssl, off=0):
                            for kc in range(KC):
                                inst = e.matmul(ps[:, 0:SGW], lhsT=w[:, kc, half * 256 + off:half * 256 + 255 + off:2], rhs=xeT[:, kc, ssl],
                                                start=(kc == 0), stop=(kc == KC - 1))
                            return inst
                        sc.op("pe", fg, reads=[wk] + xeTk, writes=[pgk])
                        sc.op("pe", lambda e, psu=psu, w=w, half=half, ssl=ssl: fg(e, psu, w, half, ssl, 1), reads=[wk] + xeTk, writes=[puk])
                        g, gk = gR.next()
                        u, uk = uR.next()
                        s_, sk = sR.next()
                        sc.op("dve", lambda e, g=g, psg=psg, bg=bg, fc=fc: e.tensor_scalar(
                            out=g[:, :], in0=psg[:, 0:SGW], scalar1=bg[:, 2 * fc:2 * fc + 1], scalar2=7.0, op0=ALU.add, op1=ALU.min),
                            reads=[pgk, bgk], writes=[gk])
                        sc.op("dve", lambda e, u=u, psu=psu, bg=bg, fc=fc: e.tensor_scalar(
                            out=u[:, :], in0=psu[:, 0:SGW], scalar1=bg[:, 2 * fc + 1:2 * fc + 2], scalar2=7.0, op0=ALU.add, op1=ALU.min),
                            reads=[puk, bgk], writes=[uk])
                        sc.op("pool", lambda e, u=u: e.tensor_scalar(out=u[:, :], in0=u[:, :], scalar1=-7.0, scalar2=1.0,
                                                                      op0=ALU.max, op1=ALU.add), reads=[uk], writes=[uk])
                        sc.op("act", lambda e, s_=s_, g=g: e.activation(out=s_[:, :], in_=g[:, :], func=AF.Sigmoid, scale=1.702),
                              reads=[gk], writes=[sk])
                        sc.op("pool", lambda e, s_=s_, g=g: e.tensor_tensor(out=g[:, :], in0=g[:, :], in1=s_[:, :], op=ALU.mult),
                              reads=[gk, sk], writes=[gk])
                        sc.op("dve", lambda e, g=g, u=u, fc=fc, ssl=ssl: e.tensor_tensor(out=actT[:, fc, ssl], in0=u[:, :], in1=g[:, :], op=ALU.mult),
                              reads=[gk, uk], writes=[("actT", fc)])
            actk = [("actT", fc) for fc in range(KC)]
            for blk in range(4):
                w, wk = wR.next()
                sc.dma("pool", w[:, :, :], d["wd"][ex][:, blk * 512:(blk + 1) * 512].rearrange("(kc p) n -> p kc n", p=128), writes=[wk])
                for st in range(NST):
                    py, pyk = psY.next()

                    def fy(e, py=py, w=w, st=st, bd=bd, blk=blk):
                        for kc in range(KC):
                            e.matmul(py[:, :], lhsT=actT[:, kc, st * 128:(st + 1) * 128], rhs=w[:, kc, :], start=(kc == 0), stop=False)
                        return e.matmul(py[:, :], lhsT=ones_f[0:1, :], rhs=bd[0:1, blk * 512:(blk + 1) * 512], start=False, stop=True)
                    sc.op("pe", fy, reads=[wk, bdk, "ones"] + actk, writes=[pyk])
                    y, yk = yR.next()
                    sc.op("act", lambda e, y=y, py=py: e.activation(out=y[:, :], in_=py[:, :], func=AF.Copy), reads=[pyk], writes=[yk])
                    r0 = ex * C + st * 128
                    sc.dma("sp", d["Y"][r0:r0 + 128, blk * 512:(blk + 1) * 512], y[:, :], reads=[yk], writes=[("Y", ex, st, blk)])
        sc.emit()


def phase_tail3(sc, nc, T, C, d):
    NT = T // 128
    with ExitStack() as ctx:
        sc.begin(ctx, "t3")
        ln_g = sc.sb("lng", [128, 2048], F32)
        ln_b = sc.sb("lnb", [128, 2048], F32)
        lneps = sc.sb("lneps", [128, 1], F32)
        pos_i = sc.sb("posi", [128, NT, 4], I32)
        gate4 = sc.sb("gate4", [128, NT, 4], F32)
        lnst = {"sum": sc.sb("lnsum", [128, 4], F32), "sq": sc.sb("lnsq", [128, 2048], BF16), "eps": lneps}
        rR = Ring([(sc.sb(f"r{i}", [128, 2048], F32), f"r{i}") for i in range(2)])
        yR = Ring([(sc.sb(f"y{i}", [128, 2048], F32), f"y{i}") for i in range(4)])
        ixR = Ring([(sc.sb(f"ix{i}", [128, 1], I32), f"ix{i}") for i in range(8)])
        sc.dma("sp", ln_g[:, :], d["ln2g"], writes=["lnconst"])
        sc.dma("sp", ln_b[:, :], d["ln2b"], writes=["lnconst"])
        sc.dma("sp", pos_i[:, :, :], d["pos_scr"], writes=["posi"])
        sc.dma("sp", gate4[:, :, :], d["gate_scr"], writes=["gate4"])
        sc.op("pool", lambda e: e.memset(lneps[:, :], LN_EPS), writes=["lneps"])
        for n in range(NT):
            r, rk = rR.next()
            sc.dma("sp", r[:, :], d["r2_scr"][n * 128:(n + 1) * 128, :], writes=[rk])
            for k in range(4):
                y, yk = yR.next()
                ix, ixk = ixR.next()
                sc.op("dve", lambda e, ix=ix, n=n, k=k: e.tensor_copy(ix[:, :], pos_i[:, n, k:k + 1]), reads=["posi"], writes=[ixk])
                sc.op("pool", lambda e, y=y, ix=ix: e.indirect_dma_start(
                    out=y[:, :], out_offset=None, in_=d["Y"][:, :],
                    in_offset=bass.IndirectOffsetOnAxis(ap=ix[:, :], axis=0)), reads=[ixk], writes=[yk], dma=True)
                sc.op("dve", lambda e, y=y, r=r, n=n, k=k: e.scalar_tensor_tensor(
                    out=r[:, :], in0=y[:, :], scalar=gate4[:, n, k:k + 1], in1=r[:, :], op0=ALU.mult, op1=ALU.add),
                    reads=[yk, rk, "gate4"], writes=[rk])
            _layernorm(sc, r[:, :], rk, 2048.0, ln_g, ln_b, lnst, "ln2", r[:, :], rk)
            sc.dma("sp", d["out"][n * 128:(n + 1) * 128, :], r[:, :], reads=[rk], writes=[("out", n)])
        sc.emit()


def build_l1(S):
    nc = bass.Bass("TRN2", target_bir_lowering=False)
    I = lambda n, shp, dt=F32: nc.dram_tensor(n, list(shp), dt, kind="ExternalInput").ap()
    O = lambda n, shp, dt=F32: nc.dram_tensor(n, list(shp), dt, kind="ExternalOutput").ap()
    X = lambda n, shp, dt=F32: nc.dram_tensor(n, list(shp), dt, kind="Internal").ap()
    xT3 = I("xT3", [3, 2048, S]); w_att = I("w_att", [12, 2048, 384]); abias = I("abias", [12, 3, 128, 128])
    ident = I("ident", [128, 128]); w_dn = I("w_dn", [2048, DN_COLS]); convT = I("convT", [2048, 5])
    gparam = I("gparam", [32, 2]); cmat = I("cmat", [6, 128, 128]); normw = I("normw", [128, 1])
    o_aT = O("o_aT", [512, S]); o_bT = O("o_bT", [1024, S])
    dn_q = X("dn_q", [4, 128, S], BF16); dn_k = X("dn_k", [4, 128, S], BF16); dn_v = X("dn_v", [8, 128, S], BF16)
    dn_zg = X("dn_zg", [8, 128, S], BF16); dn_gT = X("dn_gT", [32, S]); dn_o = X("dn_o", [8, 2, 128, S])
    with ExitStack() as gctx:
        sc = Sched(nc, gctx)
        phase_attention(sc, nc, S, xT3, w_att, abias, ident, o_aT)
        phase_dn_proj(sc, nc, S, xT3[0], w_dn, convT, dn_q, dn_k, dn_v, dn_zg, dn_gT)
        phase_dn_main(sc, nc, S, dn_q, dn_k, dn_v, dn_gT, gparam, cmat, ident, dn_o)
        phase_dn_final(sc, nc, S, dn_o, dn_zg, normw, o_bT)
    return nc


L2_INPUTS = {
    "xT": [2048, None], "x": [None, 2048], "oT": [3072, None], "pT": [256, None], "wg": [2048, 4096], "bgate": [128, 32],
    "wa": [1024, 2048], "wb": [2048, 2048], "wout": [2048, 2048], "ln1g": [128, 2048], "ln1b": [128, 2048],
    "wr": [2048, 32], "rb": [128, 32], "ebase": [128, 32], "UT": [128, 128], "ident": [128, 128],
    "wpg": [2048, 2048], "bpg": [128, 2048], "wple": [256, 2048], "wgu": [32, 2048, 4096], "bgu": [32, 128, 32],
    "wd": [32, 2048, 2048], "bd": [32, 1, 2048], "ln2g": [128, 2048], "ln2b": [128, 2048],
}


def build_l2(T, C, n_exp=N_EXPERTS):
    nc = bass.Bass("TRN2", target_bir_lowering=False)
    d = {}
    for name, shp in L2_INPUTS.items():
        shp = [T if v is None else v for v in shp]
        d[name] = nc.dram_tensor(name, shp, F32, kind="ExternalInput").ap()
    d["out"] = nc.dram_tensor("out", [T, 2048], F32, kind="ExternalOutput").ap()
    X = lambda n, shp, dt=F32: nc.dram_tensor(n, list(shp), dt, kind="Internal").ap()
    d["h_scr"] = X("h_scr", [T, 2048]); d["r2_scr"] = X("r2_scr", [T, 2048])
    d["Xs"] = X("Xs", [N_EXPERTS * C, 2048], BF16); d["Y"] = X("Y", [N_EXPERTS * C, 2048])
    d["pos_scr"] = X("pos_scr", [128, T // 128, 4], I32); d["gate_scr"] = X("gate_scr", [128, T // 128, 4])
    with ExitStack() as gctx:
        sc = Sched(nc, gctx)
        phase_tail1a(sc, nc, T, d)
        phase_tail1b(sc, nc, T, C, d)
        phase_experts(sc, nc, C, d, n_exp)
        phase_tail3(sc, nc, T, C, d)
    return nc


A_QKV = 3072
OFF_QB = 9216
OFF_KB = 10240
OFF_VB = 11264
OFF_ZB = 13312
OFF_BA = 15360
OFF_G = 15424
_CACHE = {}


def _rep(v, n=128):
    return np.ascontiguousarray(np.broadcast_to(np.asarray(v, np.float32).reshape(1, -1), (n, np.asarray(v).size)))


def l1_inputs(x_b, w_in, conv_w, a_log, dt_bias, dn_norm_w, hf):
    xT = np.ascontiguousarray(x_b.T)
    xT3 = np.stack([permute_tokens(xT, r) for r in DILS])
    w_att = np.zeros((12, 2048, 384), np.float32)
    for hl in range(4):
        for g in range(3):
            h = hf * 4 + hl
            for j in range(3):
                c0 = j * A_QKV + g * 1024 + h * 128
                w_att[hl * 3 + g][:, j * 128:(j + 1) * 128] = w_in[:, c0:c0 + 128]
    w_dn = np.concatenate(
        [w_in[:, OFF_QB + hf * 512:OFF_QB + (hf + 1) * 512], w_in[:, OFF_KB + hf * 512:OFF_KB + (hf + 1) * 512],
         w_in[:, OFF_VB + hf * 1024:OFF_VB + (hf + 1) * 1024], w_in[:, OFF_ZB + hf * 1024:OFF_ZB + (hf + 1) * 1024]]
        + [w_in[:, OFF_BA + i * 16 + hf * 8:OFF_BA + i * 16 + hf * 8 + 8] for i in range(4)], axis=1)
    cq = conv_w[:, 0:1024][:, hf * 512:(hf + 1) * 512]
    ck = conv_w[:, 1024:2048][:, hf * 512:(hf + 1) * 512]
    cv = conv_w[:, 2048:4096][:, hf * 1024:(hf + 1) * 1024]
    convT = np.ascontiguousarray(np.concatenate([cq, ck, cv], 1).T)
    gparam = np.zeros((32, 2), np.float32)
    gparam[8:16, 0] = dt_bias[0, hf * 8:hf * 8 + 8]
    gparam[8:16, 1] = a_log[0, hf * 8:hf * 8 + 8]
    gparam[24:32, 0] = dt_bias[1, hf * 8:hf * 8 + 8]
    gparam[24:32, 1] = a_log[1, hf * 8:hf * 8 + 8]
    return {"xT3": xT3, "w_att": w_att, "abias": make_abias(hf), "ident": np.eye(128, dtype=np.float32),
            "w_dn": np.ascontiguousarray(w_dn), "convT": convT, "gparam": gparam, "cmat": make_cmat(),
            "normw": np.asarray(dn_norm_w, np.float32).reshape(128, 1)}


def l2_shared(w_in, b_gate, w_branch_a, w_branch_b, w_out, ln1_g, ln1_b, router_w, router_b, w_gate_up, b_gate_up,
              w_down, b_down, w_ple, w_ple_gate, b_ple_gate, ln2_g, ln2_b, C):
    i = np.arange(128)[:, None]
    j = np.arange(128)[None, :]
    bgu = np.ascontiguousarray(b_gate_up.reshape(32, 16, 128, 2).transpose(0, 2, 1, 3).reshape(32, 128, 32))
    return {
        "wg": np.ascontiguousarray(w_in[:, OFF_G:OFF_G + 4096]),
        "bgate": np.ascontiguousarray(b_gate.reshape(32, 128).T),
        "wa": w_branch_a, "wb": w_branch_b, "wout": w_out, "ln1g": _rep(ln1_g), "ln1b": _rep(ln1_b),
        "wr": router_w, "rb": _rep(router_b), "ebase": _rep(np.arange(32, dtype=np.float32) * C),
        "UT": (i < j).astype(np.float32), "ident": np.eye(128, dtype=np.float32),
        "wpg": w_ple_gate, "bpg": _rep(b_ple_gate), "wple": w_ple, "wgu": w_gate_up, "bgu": bgu,
        "wd": w_down, "bd": np.ascontiguousarray(b_down.reshape(32, 1, 2048)), "ln2g": _rep(ln2_g), "ln2b": _rep(ln2_b),
    }


def kernel(x, p, w_in, b_gate, conv_w, a_log, dt_bias, dn_norm_w, w_branch_a, w_branch_b, w_out, ln1_g, ln1_b,
           router_w, router_b, w_gate_up, b_gate_up, w_down, b_down, w_ple, w_ple_gate, b_ple_gate, ln2_g, ln2_b):
    f = lambda a: np.asarray(a, np.float32)
    x = f(x); p = f(p)[0]
    (w_in, b_gate, conv_w, a_log, dt_bias, dn_norm_w, w_branch_a, w_branch_b, w_out, ln1_g, ln1_b, router_w, router_b,
     w_gate_up, b_gate_up, w_down, b_down, w_ple, w_ple_gate, b_ple_gate, ln2_g, ln2_b) = [
        f(a)[0] for a in (w_in, b_gate, conv_w, a_log, dt_bias, dn_norm_w, w_branch_a, w_branch_b, w_out, ln1_g, ln1_b,
                          router_w, router_b, w_gate_up, b_gate_up, w_down, b_down, w_ple, w_ple_gate, b_ple_gate, ln2_g, ln2_b)]
    B, S, _ = x.shape
    NCORE = 8
    T = B * S // NCORE
    C = 1024
    if "l1" not in _CACHE:
        _CACHE["l1"] = build_l1(S)
    maps1 = [l1_inputs(x[c // 2], w_in, conv_w, a_log, dt_bias, dn_norm_w, c % 2) for c in range(NCORE)]
    res1 = run_bass_kernel_spmd(_CACHE["l1"], maps1, core_ids=list(range(NCORE))).results
    del maps1
    if "l2" not in _CACHE:
        _CACHE["l2"] = build_l2(T, C)
    shared = l2_shared(w_in, b_gate, w_branch_a, w_branch_b, w_out, ln1_g, ln1_b, router_w, router_b, w_gate_up, b_gate_up,
                       w_down, b_down, w_ple, w_ple_gate, b_ple_gate, ln2_g, ln2_b, C)
    maps2 = []
    per_b = S // T
    for c in range(NCORE):
        b = c // per_b
        s0 = (c % per_b) * T
        oT = np.concatenate([res1[2 * b]["o_aT"][:, s0:s0 + T], res1[2 * b + 1]["o_aT"][:, s0:s0 + T],
                             res1[2 * b]["o_bT"][:, s0:s0 + T], res1[2 * b + 1]["o_bT"][:, s0:s0 + T]], axis=0)
        m = dict(shared)
        m["xT"] = np.ascontiguousarray(x[b, s0:s0 + T].T)
        m["x"] = np.ascontiguousarray(x[b, s0:s0 + T])
        m["oT"] = np.ascontiguousarray(oT)
        m["pT"] = np.ascontiguousarray(p[b, s0:s0 + T].T)
        maps2.append(m)
    res2 = run_bass_kernel_spmd(_CACHE["l2"], maps2, core_ids=list(range(NCORE))).results
    out = np.stack([r["out"] for r in res2]).reshape(B, S, 2048)
    return out.astype(np.float32)
```
